# Optimizing a Trainium2 kernel written in Bass

```python
import jax, jax.numpy as jnp
from jax import lax
import numpy as np


D_MODEL = 1024
BATCH = 8
SEQ = 2048
DEPTH = 4

GM_GROUPS = 8
GM_GROUP_DIM = D_MODEL // 16
GM_WIDTH = GM_GROUPS * GM_GROUP_DIM
GM_CHUNK = 128
ATTN_HEADS = 8
HEAD_DIM = D_MODEL // 16
ATTN_WIDTH = ATTN_HEADS * HEAD_DIM
MOBA_BLOCK = 256
MOBA_TOPK = 3
MOBA_QCHUNK = 32
ROPE_THETA = 10000.0
D_FF = -(-8 * D_MODEL // (3 * 256)) * 256
IN_WIDTH = 2 * GM_WIDTH + 3 * ATTN_WIDTH + 2 * D_MODEL
NORM_EPS = 1e-6
NEG_INF = -1e30

kernel_name = 'hybrid_gmlp_moba_block'


def rms_norm(x, g):
    xf = x.astype(jnp.float32)
    y = xf * lax.rsqrt(jnp.mean(xf * xf, axis=-1, keepdims=True) + NORM_EPS)
    return (y * g.astype(jnp.float32)).astype(x.dtype)


def layer_norm(x, g, b):
    xf = x.astype(jnp.float32)
    mu = jnp.mean(xf, axis=-1, keepdims=True)
    var = jnp.mean(jnp.square(xf - mu), axis=-1, keepdims=True)
    y = (xf - mu) * lax.rsqrt(var + NORM_EPS)
    return (y * g.astype(jnp.float32) + b.astype(jnp.float32)).astype(x.dtype)


def rope_tables(positions):
    inv_freq = 1.0 / (ROPE_THETA ** (jnp.arange(0, HEAD_DIM, 2, dtype=jnp.float32) / HEAD_DIM))
    ang = positions.astype(jnp.float32)[..., None] * inv_freq
    return jnp.cos(ang)[:, :, None, :], jnp.sin(ang)[:, :, None, :]


def apply_rope(x, cos, sin):
    xf = x.astype(jnp.float32)
    x1, x2 = jnp.split(xf, 2, axis=-1)
    return jnp.concatenate([x1 * cos - x2 * sin, x2 * cos + x1 * sin], axis=-1).astype(x.dtype)


def spatial_gating_unit(u, v, w_s, b_s, ln_g, ln_b):
    B, S, _ = v.shape
    n_chunks = S // GM_CHUNK
    v = layer_norm(v, ln_g, ln_b)
    vc = v.reshape(B, n_chunks, GM_CHUNK, GM_GROUPS, GM_GROUP_DIM)
    causal = jnp.tril(jnp.ones((GM_CHUNK, GM_CHUNK), dtype=bool))
    w = jnp.where(causal, w_s, jnp.zeros((), w_s.dtype))
    mixed = jnp.einsum('gij,bcjgd->bcigd', w, vc) + b_s.T[None, None, :, :, None]
    return u * mixed.reshape(B, S, GM_WIDTH)


def moba_attention(q, k, v):
    B, H, S, hd = q.shape
    s_pad = -(-S // MOBA_BLOCK) * MOBA_BLOCK
    pad = ((0, 0), (0, 0), (0, s_pad - S), (0, 0))
    q = jnp.pad(q, pad)
    k = jnp.pad(k, pad)
    v = jnp.pad(v, pad)
    n_blocks = s_pad // MOBA_BLOCK
    topk = max(1, min(MOBA_TOPK, n_blocks - 1))
    scale = HEAD_DIM ** -0.5

    kb = k.reshape(B, H, n_blocks, MOBA_BLOCK, hd)
    vb = v.reshape(B, H, n_blocks, MOBA_BLOCK, hd)
    kbar = jnp.mean(kb.astype(jnp.float32), axis=3)
    gate = jnp.einsum('bhsd,bhnd->bhsn', q.astype(jnp.float32), kbar)
    q_block = jnp.arange(s_pad) // MOBA_BLOCK
    fully_past = jnp.arange(n_blocks)[None, :] < q_block[:, None]
    gate = jnp.where(fully_past, gate, -jnp.inf)
    _, idx = lax.top_k(gate, topk)
    valid = idx < q_block[:, None]

    n_q = s_pad // MOBA_QCHUNK
    qc = q.reshape(B, H, n_q, MOBA_QCHUNK, hd).transpose(2, 0, 1, 3, 4)
    idxc = idx.reshape(B, H, n_q, MOBA_QCHUNK, topk).transpose(2, 0, 1, 3, 4)
    validc = valid.reshape(B, H, n_q, MOBA_QCHUNK, topk).transpose(2, 0, 1, 3, 4)
    bi = jnp.arange(B)[:, None, None, None]
    hi = jnp.arange(H)[None, :, None, None]

    def step(args):
        c, qq, ii, vv = args
        kg = kb[bi, hi, ii]
        vg = vb[bi, hi, ii]
        s_sel = jnp.einsum('bhqd,bhqnkd->bhqnk', qq, kg, preferred_element_type=jnp.float32) * scale
        s_sel = jnp.where(vv[..., None], s_sel, NEG_INF).reshape(B, H, MOBA_QCHUNK, topk * MOBA_BLOCK)
        blk = (c * MOBA_QCHUNK) // MOBA_BLOCK
        k_own = lax.dynamic_index_in_dim(kb, blk, axis=2, keepdims=False)
        v_own = lax.dynamic_index_in_dim(vb, blk, axis=2, keepdims=False)
        s_own = jnp.einsum('bhqd,bhkd->bhqk', qq, k_own, preferred_element_type=jnp.float32) * scale
        q_pos = c * MOBA_QCHUNK + jnp.arange(MOBA_QCHUNK)
        k_pos = blk * MOBA_BLOCK + jnp.arange(MOBA_BLOCK)
        s_own = jnp.where(k_pos[None, :] <= q_pos[:, None], s_own, NEG_INF)
        p = jax.nn.softmax(jnp.concatenate([s_sel, s_own], axis=-1), axis=-1).astype(v.dtype)
        p_sel = p[..., :topk * MOBA_BLOCK].reshape(B, H, MOBA_QCHUNK, topk, MOBA_BLOCK)
        p_own = p[..., topk * MOBA_BLOCK:]
        return (jnp.einsum('bhqnk,bhqnkd->bhqd', p_sel, vg)
                + jnp.einsum('bhqk,bhkd->bhqd', p_own, v_own))

    out = lax.map(step, (jnp.arange(n_q), qc, idxc, validc))
    out = out.transpose(1, 2, 0, 3, 4).reshape(B, H, s_pad, hd)
    return out[:, :, :S]


def hybrid_layer(x, cos, sin, w_in, w_s, b_s, ln_v_g, ln_v_b, w_proj_a, w_proj_b, w_out,
                 g_mix_pre, g_mix_post, g_ffn_pre, g_ffn_post, w_gate_up, w_down):
    B, S, _ = x.shape
    h = rms_norm(x, g_mix_pre)
    z = h @ w_in
    o1 = GM_WIDTH
    o2 = 2 * GM_WIDTH
    o3 = o2 + ATTN_WIDTH
    o4 = o3 + ATTN_WIDTH
    o5 = o4 + ATTN_WIDTH
    o6 = o5 + D_MODEL
    u, vg, q, k, va, gate_a, gate_b = jnp.split(z, [o1, o2, o3, o4, o5, o6], axis=-1)
    y_a = spatial_gating_unit(jax.nn.gelu(u), jax.nn.gelu(vg), w_s, b_s, ln_v_g, ln_v_b)
    q = apply_rope(q.reshape(B, S, ATTN_HEADS, HEAD_DIM), cos, sin).transpose(0, 2, 1, 3)
    k = apply_rope(k.reshape(B, S, ATTN_HEADS, HEAD_DIM), cos, sin).transpose(0, 2, 1, 3)
    va = va.reshape(B, S, ATTN_HEADS, HEAD_DIM).transpose(0, 2, 1, 3)
    y_b = moba_attention(q, k, va).transpose(0, 2, 1, 3).reshape(B, S, ATTN_WIDTH)
    merged = jax.nn.sigmoid(gate_a) * (y_a @ w_proj_a) + jax.nn.sigmoid(gate_b) * (y_b @ w_proj_b)
    x = x + rms_norm(merged @ w_out, g_mix_post)
    h = rms_norm(x, g_ffn_pre)
    gate, up = jnp.split(h @ w_gate_up, 2, axis=-1)
    f = (jax.nn.silu(gate) * up) @ w_down
    return x + rms_norm(f, g_ffn_post)


def setup_inputs(seed: int = 0) -> dict:
    key = jax.random.key(seed)
    ks = jax.random.split(key, 16)
    f32 = jnp.float32

    def nrm(k_, shape, fan_in):
        return jax.random.normal(k_, shape, f32) * (fan_in ** -0.5)

    def gain(k_, shape):
        return 1.0 + 0.02 * jax.random.normal(k_, shape, f32)

    x = jax.random.normal(ks[0], (BATCH, SEQ, D_MODEL), f32)
    positions = jnp.broadcast_to(jnp.arange(SEQ, dtype=jnp.int32), (BATCH, SEQ))
    return {
        'x': x,
        'positions': positions,
        'w_in': nrm(ks[1], (DEPTH, D_MODEL, IN_WIDTH), D_MODEL),
        'w_s': nrm(ks[2], (DEPTH, GM_GROUPS, GM_CHUNK, GM_CHUNK), GM_CHUNK),
        'b_s': gain(ks[3], (DEPTH, GM_GROUPS, GM_CHUNK)),
        'ln_v_g': gain(ks[4], (DEPTH, GM_WIDTH)),
        'ln_v_b': 0.02 * jax.random.normal(ks[5], (DEPTH, GM_WIDTH), f32),
        'w_proj_a': nrm(ks[6], (DEPTH, GM_WIDTH, D_MODEL), GM_WIDTH),
        'w_proj_b': nrm(ks[7], (DEPTH, ATTN_WIDTH, D_MODEL), ATTN_WIDTH),
        'w_out': nrm(ks[8], (DEPTH, D_MODEL, D_MODEL), D_MODEL),
        'g_mix_pre': gain(ks[9], (DEPTH, D_MODEL)),
        'g_mix_post': gain(ks[10], (DEPTH, D_MODEL)),
        'g_ffn_pre': gain(ks[11], (DEPTH, D_MODEL)),
        'g_ffn_post': gain(ks[12], (DEPTH, D_MODEL)),
        'w_gate_up': nrm(ks[13], (DEPTH, D_MODEL, 2 * D_FF), D_MODEL),
        'w_down': nrm(ks[14], (DEPTH, D_FF, D_MODEL), D_FF),
    }


def reference(x, positions, w_in, w_s, b_s, ln_v_g, ln_v_b, w_proj_a, w_proj_b, w_out,
              g_mix_pre, g_mix_post, g_ffn_pre, g_ffn_post, w_gate_up, w_down):
    cos, sin = rope_tables(positions)
    for l in range(DEPTH):
        x = hybrid_layer(x, cos, sin, w_in[l], w_s[l], b_s[l], ln_v_g[l], ln_v_b[l],
                         w_proj_a[l], w_proj_b[l], w_out[l], g_mix_pre[l], g_mix_post[l],
                         g_ffn_pre[l], g_ffn_post[l], w_gate_up[l], w_down[l])
    return x
```

```python
import itertools
import math
import numpy as np
import concourse.bass as bass
import concourse.mybir as mybir
from concourse.bass_utils import run_bass_kernel_spmd

F32 = mybir.dt.float32
BF16 = mybir.dt.bfloat16
U8 = mybir.dt.uint8
I32 = mybir.dt.int32
AF = mybir.ActivationFunctionType
ALU = mybir.AluOpType
AX = mybir.AxisListType

D = 1024
S = 2048
NT = S // 128
DEPTH = 4
NCORES = 8
GMW = 512
AW = 512
DFF = 2816
NFF = DFF // 128
INW = 2 * GMW + 3 * AW + 2 * D
O_U, O_VG, O_Q, O_K, O_VA, O_GA, O_GB = 0, 512, 1024, 1536, 2048, 2560, 3584
EPS = 1e-6
NEG = -30000.0
TWO_PI = 2.0 * math.pi


def _dsize(dt):
    return mybir.dt.size(dt)


class _Op:
    __slots__ = ("idx", "q", "stream", "fn", "waits", "signal", "clock", "sigval", "grp", "isdma")


class _Grp:
    def __init__(self):
        self.ops = []


class Prog:
    def __init__(self, nc):
        self.nc = nc
        self.ops = []
        self.lastw = {}
        self.readers = {}
        self.known = {}
        self.gran = {}
        self.engs = {"pe": nc.tensor, "act": nc.scalar, "dve": nc.vector, "pool": nc.gpsimd, "sp": nc.sync}
        self._fcache = {}

    def chunks(self, ap):
        name = ap.tensor.name
        g = self.gran.get(name)
        if g is None:
            return ()
        pairs = tuple(ap.ap)
        key = (name, ap.offset, pairs, ap.dtype)
        r = self._fcache.get(key)
        if r is not None:
            return r
        es = _dsize(ap.dtype)
        rowstride = pairs[0][0]
        off = ap.offset % rowstride
        free = pairs[1:]
        if not free:
            free = ((1, 1),)
        outer = free[:-1]
        lstep, lcnt = free[-1]
        llen = ((lcnt - 1) * lstep + 1) if lstep != 0 else 1
        res = set()
        for idxs in itertools.product(*[range(c) for (_, c) in outer]):
            st = off + sum(s * i for (s, _), i in zip(outer, idxs))
            b0 = st * es
            b1 = (st + llen) * es - 1
            for c in range(b0 // g, b1 // g + 1):
                res.add((name, c))
        r = tuple(res)
        self._fcache[key] = r
        return r

    def add(self, q, fn, reads=(), writes=(), dsem=None, grp=None):
        op = _Op()
        op.idx = len(self.ops)
        op.q = q
        op.isdma = dsem is not None
        op.stream = ("D:" + dsem) if dsem is not None else q
        op.fn = fn
        op.signal = False
        op.sigval = None
        op.grp = grp
        if grp is not None:
            grp.ops.append(op)
        rch = set()
        for a in reads:
            rch.update(self.chunks(a))
        wch = set()
        for a in writes:
            wch.update(self.chunks(a))
        deps = {}

        def need(i):
            t = self.ops[i]
            if t.grp is not None:
                t = t.grp.ops[-1]
            s = t.stream
            if deps.get(s, -1) < t.idx:
                deps[s] = t.idx

        for c in rch:
            w = self.lastw.get(c)
            if w is not None:
                need(w)
        for c in wch:
            w = self.lastw.get(c)
            if w is not None:
                need(w)
            rd = self.readers.get(c)
            if rd:
                for i in rd.values():
                    need(i)
        kn = self.known.setdefault(q, {})
        waits = []
        for s, i in deps.items():
            if s == "pe" and q == "pe" and not op.isdma:
                continue
            if i == op.idx:
                continue
            if kn.get(s, -1) >= i:
                continue
            t = self.ops[i]
            t.signal = True
            waits.append(t)
            for s2, i2 in t.clock.items():
                if kn.get(s2, -1) < i2:
                    kn[s2] = i2
        op.waits = waits
        op.clock = dict(kn)
        op.clock[op.stream] = op.idx
        for c in rch:
            self.readers.setdefault(c, {})[op.stream] = op.idx
        for c in wch:
            self.lastw[c] = op.idx
            self.readers[c] = {}
        self.ops.append(op)
        return op

    def emit(self):
        nc = self.nc
        handles = {}
        counts = {}

        def H(s):
            if s not in handles:
                handles[s] = nc.alloc_semaphore("s_" + s.replace(":", "_"))
                counts[s] = 0
            return handles[s]

        for op in self.ops:
            eng = self.engs[op.q]
            wl = {}
            for t in op.waits:
                assert t.sigval is not None, (op.idx, t.idx, t.stream)
                if wl.get(t.stream, -1) < t.sigval:
                    wl[t.stream] = t.sigval
            wl = list(wl.items())
            for s, v in wl[:-1]:
                eng.wait_ge(H(s), v)
            ins = op.fn()
            if wl:
                s, v = wl[-1]
                ins._wait_ge(H(s), v)
            if op.isdma:
                h = H(op.stream)
                counts[op.stream] += 16
                ins.then_inc(h, 16)
                op.sigval = counts[op.stream]
            elif op.signal:
                h = H(op.stream)
                counts[op.stream] += 1
                ins.then_inc(h, 1)
                op.sigval = counts[op.stream]
        return len(handles)


class _Stop(Exception):
    pass


def build_program(nlayers, dbg=None, stop=None):
    nc = bass.Bass("TRN2", target_bir_lowering=False)
    L = nlayers

    def din(name, shape, dt=F32):
        return nc.dram_tensor(name, shape, dt, kind="ExternalInput").ap()

    x_in = din("x", [S, D])
    pos_in = din("positions", [S], I32)
    w_in = din("w_in", [L, D, INW])
    w_s = din("w_s", [L, 8, 128, 128])
    b_s = din("b_s", [L, 8, 128])
    ln_g = din("ln_v_g", [L, GMW])
    ln_b = din("ln_v_b", [L, GMW])
    w_pa = din("w_proj_a", [L, GMW, D])
    w_pb = din("w_proj_b", [L, AW, D])
    w_out = din("w_out", [L, D, D])
    g_mpre = din("g_mix_pre", [L, D])
    g_mpost = din("g_mix_post", [L, D])
    g_fpre = din("g_ffn_pre", [L, D])
    g_fpost = din("g_ffn_post", [L, D])
    w_gu = din("w_gate_up", [L, D, 2 * DFF])
    w_dn = din("w_down", [L, DFF, D])
    out = nc.dram_tensor("out", [S, D], F32, kind="ExternalOutput").ap()
    dbg_outs = {}

    P = Prog(nc)

    XB = 65536
    BIGB = 106496
    RINGB = 3 * 8192
    SMB = 16128
    Xa = nc.alloc_sbuf_tensor("X", [128, XB], U8)[:]
    Ba = nc.alloc_sbuf_tensor("B", [128, BIGB], U8)[:]
    Ra = nc.alloc_sbuf_tensor("R", [128, RINGB], U8)[:]
    Sa = nc.alloc_sbuf_tensor("SM", [128, SMB], U8)[:]
    PSa = nc.alloc_psum_tensor("PS", [128, 4096], F32)[:]
    P.gran = {"X": 2048, "B": 256, "R": 512, "SM": 32, "PS": 2048}

    def view(arena, off, dt, shape):
        n = int(np.prod(shape)) * _dsize(dt)
        v = arena[:, off:off + n].bitcast(dt)
        if len(shape) == 2:
            v = v.rearrange("p (a b) -> p a b", b=shape[1])
        elif len(shape) == 3:
            v = v.rearrange("p (a b c) -> p a b c", b=shape[1], c=shape[2])
        return v

    X = view(Xa, 0, F32, [NT, D])

    QT = view(Ba, 0, BF16, [4, S])
    KT = view(Ba, 16384, BF16, [4, S])
    VA = view(Ba, 32768, BF16, [NT, 8, 72])
    R1 = 51200
    HT = view(Ba, R1, BF16, [8, S])
    PTB = [[view(Ba, R1 + (u * 2 + p_) * 8192, BF16, [16, 256]) for p_ in range(2)] for u in range(2)]
    R3 = R1 + 32768
    YBT = view(Ba, R3, BF16, [4, S])
    R4 = R3 + 16384
    MBT = view(Ba, R4, BF16, [1024])
    YBTOK = [view(Ba, R4 + 2048, BF16, [2, 512]), view(Ba, R4 + 4096, BF16, [2, 512])]
    RT1 = [view(Ba, R3, F32, [512]), view(Ba, R3 + 2048, F32, [512])]
    RT2 = [view(Ba, R3 + 4096, F32, [512]), view(Ba, R3 + 6144, F32, [512])]
    QR = [view(Ba, R3 + 8192 + 1024 * i, BF16, [512]) for i in range(4)]
    BSB = view(Ba, 0, F32, [4, 128])
    BIAS2 = view(Ba, 2048, F32, [4, 128])
    WST = view(Ba, 4096, BF16, [8, 128])
    WSS = view(Ba, 8192, F32, [8, 128])
    WS16 = view(Ba, 12288, BF16, [8, 128])
    G32 = [view(Ba, 8192 + 2048 * i, F32, [512]) for i in range(8)]
    V16 = [view(Ba, 24576 + 1024 * i, BF16, [512]) for i in range(4)]
    UT = view(Ba, 32768, BF16, [4, S])
    MT = view(Ba, 0, BF16, [8, S])
    SGA = view(Ba, R4, BF16, [512])
    SGB = view(Ba, R4 + 1024, BF16, [512])
    T1 = view(Ba, R4 + 2048, F32, [512])
    T2 = view(Ba, R4 + 4096, F32, [512])
    TMPC = [view(Ba, R3, F32, [512]), view(Ba, R3 + 2048, F32, [512])]
    TMPCW = [view(Ba, R3, F32, [1024]), view(Ba, R3 + 4096, F32, [1024])]
    ACTT = view(Ba, 0, BF16, [NFF, 1024])
    HTH = view(Ba, 45056, BF16, [8, 1024])
    WD = view(Ba, 61440, BF16, [NFF, 1024])
    TMPF = None
    SU = 0

    SLOT = [view(Ra, i * 8192, BF16, [4096]) for i in range(3)]
    ring_n = [0]

    def next_slot():
        s = ring_n[0] % 3
        ring_n[0] += 1
        return s

    so = [0]
    soff = {}

    def salloc(dt, shape, align=32, name=None):
        n = int(np.prod(shape)) * _dsize(dt)
        so[0] = (so[0] + align - 1) // align * align
        v = view(Sa, so[0], dt, shape)
        if name is not None:
            soff[name] = so[0]
        so[0] += n
        return v

    GPOST = salloc(F32, [D])
    XN = salloc(BF16, [D])
    TMPF = [XN.bitcast(F32), XN.bitcast(F32)]
    COS = salloc(F32, [NT, 32])
    SIN = salloc(F32, [NT, 32])
    SL = [salloc(BF16, [512], name="SL0"), salloc(BF16, [512])]
    IDENT = salloc(BF16, [128])
    TRI = salloc(BF16, [128])
    MASK01 = salloc(BF16, [128])
    GPRE = salloc(F32, [2, 8])
    SS = salloc(F32, [NT])
    SSF = salloc(F32, [NT])
    RTMP = salloc(F32, [NT])
    RTMP2 = salloc(F32, [NT])
    RSTD_MIX = salloc(F32, [NT])
    RSTD_F = salloc(F32, [NT])
    SSP = salloc(F32, [NT])
    PT1 = salloc(F32, [NT])
    PT2 = salloc(F32, [NT])
    RSTDP = salloc(F32, [NT])
    BNST = [salloc(F32, [6]) for _ in range(8)]
    MV8 = salloc(F32, [8, 2])
    LNT4 = salloc(F32, [8])
    LNT24 = salloc(F32, [8])
    LNR4 = salloc(F32, [8])
    LNGc = salloc(F32, [4])
    LNBc = salloc(F32, [4])
    ONES16 = salloc(BF16, [64])
    GSB = salloc(F32, [8, 8])
    TOP8 = salloc(F32, [8, 8])
    CMP = salloc(F32, [8, 8])
    MBTOK = salloc(BF16, [128])
    KBAR = salloc(F32, [4, 8])
    KBAR2 = salloc(F32, [4, 8])
    KBH = salloc(BF16, [4, 8])
    KBL = salloc(BF16, [4, 8])
    REC = [salloc(F32, [2]), salloc(F32, [2])]
    assert so[0] <= SMB, so[0]
    JUNK = view(Sa, soff["SL0"], BF16, [1024])

    bank_n = [0]
    pair_n = [0]

    live_pair = [None]

    def bank():
        while True:
            b = bank_n[0] % 8
            bank_n[0] += 1
            if live_pair[0] is None or (b // 2) not in live_pair[0]:
                break
        return PSa[:, b * 512:(b + 1) * 512]

    def bank_pair(keep=2):
        b = pair_n[0] % 4
        pair_n[0] += 1
        lp = (live_pair[0] or []) + [b]
        live_pair[0] = lp[-keep:]
        return PSa[:, b * 1024:(b + 1) * 1024]

    def bf(ps):
        return ps.bitcast(BF16)

    def mm(out_, lhsT, rhs, start=True, stop=True):
        P.add("pe", lambda: nc.tensor.matmul(out_, lhsT=lhsT, rhs=rhs, start=start, stop=stop),
              reads=[lhsT, rhs], writes=[out_])

    def tr(out_, in_):
        idn = IDENT[0:in_.partition_size(), 0:in_.partition_size()]
        P.add("pe", lambda: nc.tensor.transpose(out_, in_, idn), reads=[in_, idn], writes=[out_])

    def act(out_, in_, func, scale=None, bias=None, accum=None, q="act"):
        reads = [in_]
        kw = {}
        if scale is not None:
            kw["scale"] = scale
            if not isinstance(scale, (int, float)):
                reads.append(scale)
        if bias is not None:
            kw["bias"] = bias
            if not isinstance(bias, (int, float)):
                reads.append(bias)
        writes = [out_]
        if accum is not None:
            kw["accum_out"] = accum
            writes.append(accum)
        P.add(q, lambda: nc.scalar.activation(out=out_, in_=in_, func=func, **kw), reads=reads, writes=writes)

    def E(q):
        return P.engs[q]

    def tt(out_, in0, in1, op, q="dve"):
        P.add(q, lambda: E(q).tensor_tensor(out=out_, in0=in0, in1=in1, op=op), reads=[in0, in1], writes=[out_])

    def ts(out_, in0, s1, op0, s2=None, op1=None, q="dve"):
        reads = [in0]
        if not isinstance(s1, (int, float)):
            reads.append(s1)
        if s2 is not None and not isinstance(s2, (int, float)):
            reads.append(s2)
        if op1 is None:
            P.add(q, lambda: E(q).tensor_scalar(out=out_, in0=in0, scalar1=s1, scalar2=None, op0=op0),
                  reads=reads, writes=[out_])
        else:
            P.add(q, lambda: E(q).tensor_scalar(out=out_, in0=in0, scalar1=s1, scalar2=s2, op0=op0, op1=op1),
                  reads=reads, writes=[out_])

    def stt(out_, in0, scalar, in1, op0, op1, q="dve"):
        reads = [in0, in1]
        if not isinstance(scalar, (int, float)):
            reads.append(scalar)
        P.add(q, lambda: E(q).scalar_tensor_tensor(out=out_, in0=in0, scalar=scalar, in1=in1, op0=op0, op1=op1),
              reads=reads, writes=[out_])

    def cp(out_, in_, q="dve"):
        P.add(q, lambda: E(q).tensor_copy(out=out_, in_=in_), reads=[in_], writes=[out_])

    def memset(out_, val, q="dve"):
        P.add(q, lambda: E(q).memset(out_, val), writes=[out_])

    def recip(out_, in_):
        P.add("dve", lambda: nc.vector.reciprocal(out=out_, in_=in_), reads=[in_], writes=[out_])

    def dma(q, out_, in_, sem, grp, noncontig=False):
        if noncontig:
            fn = lambda: E(q).dma_start(out=out_, in_=in_, allow_slow_non_contiguous=True)
        else:
            fn = lambda: E(q).dma_start(out=out_, in_=in_)
        P.add(q, fn, reads=[in_], writes=[out_], dsem=sem, grp=grp)

    def dump(name, v, shape):
        if dbg is None or name not in dbg:
            return
        o = nc.dram_tensor("dbg_" + name, [128] + list(shape), v.dtype, kind="ExternalOutput").ap()
        dbg_outs[name] = o
        g = _Grp()
        dma("sp", o, v, "dbg_" + name, g)

    TF = view(Ba, SU, F32, [128])
    memset(TF, 0.0, q="pool")
    P.add("pool", lambda: nc.gpsimd.affine_select(out=TF, in_=TF, pattern=[[-1, 128]], compare_op=ALU.not_equal,
                                                    fill=1.0, base=0, channel_multiplier=1), reads=[TF], writes=[TF])
    cp(IDENT, TF)
    TF2 = view(Ba, SU + 512, F32, [128])
    memset(TF2, 0.0, q="pool")
    P.add("pool", lambda: nc.gpsimd.affine_select(out=TF2, in_=TF2, pattern=[[1, 128]], compare_op=ALU.is_ge,
                                                    fill=NEG, base=0, channel_multiplier=-1), reads=[TF2], writes=[TF2])
    cp(TRI, TF2)
    TF3 = view(Ba, SU + 1024, F32, [128])
    memset(TF3, 1.0, q="pool")
    P.add("pool", lambda: nc.gpsimd.affine_select(out=TF3, in_=TF3, pattern=[[1, 128]], compare_op=ALU.is_ge,
                                                    fill=0.0, base=0, channel_multiplier=-1), reads=[TF3], writes=[TF3])
    cp(MASK01, TF3)
    memset(ONES16, 1.0)

    PI_ = view(Ba, SU + 2048, I32, [NT])
    PF = view(Ba, SU + 2048 + 64, F32, [NT])
    gpos = _Grp()
    for t in range(NT):
        dma("sp", PI_[:, t:t + 1], pos_in[t * 128:(t + 1) * 128].rearrange("(p o) -> p o", o=1), "pos", gpos)
    gx = [_Grp() for _ in range(8)]
    for t in range(NT):
        dma("sp", X[:, t, :], x_in[t * 128:(t + 1) * 128, :], "xld%d" % (t // 2), gx[t // 2])
    cp(PF, PI_)
    IO = view(Ba, SU + 2304, F32, [32])
    P.add("pool", lambda: nc.gpsimd.iota(IO, pattern=[[1, 32]], base=0, channel_multiplier=0,
                                          allow_small_or_imprecise_dtypes=True), writes=[IO])
    INVF = view(Ba, SU + 2560, F32, [32])
    act(INVF, IO, AF.Exp, scale=-math.log(10000.0) / 32.0)
    ANG = view(Ba, SU + 4096, F32, [NT, 32])
    tt(ANG, PF.unsqueeze(2).to_broadcast([128, NT, 32]), INVF.unsqueeze(1).to_broadcast([128, NT, 32]), ALU.mult)
    A2 = view(Ba, SU + 6144, F32, [NT, 32])
    KF = view(Ba, SU + 8192, F32, [NT, 32])
    KI = view(Ba, SU + 10240, I32, [NT, 32])
    YR = view(Ba, SU + 12288, F32, [NT, 32])
    M1 = view(Ba, SU + 14336, F32, [NT, 32])
    for tab, shift in ((SIN, 0.0), (COS, math.pi / 2.0)):
        ts(A2, ANG, shift, ALU.add)
        ts(KF, A2, 1.0 / TWO_PI, ALU.mult)
        cp(KI, KF)
        cp(KF, KI)
        stt(YR, KF, -TWO_PI, A2, ALU.mult, ALU.add)
        ts(M1, YR, math.pi, ALU.is_gt)
        stt(YR, M1, -TWO_PI, YR, ALU.mult, ALU.add)
        ts(M1, YR, -math.pi, ALU.is_lt)
        stt(YR, M1, TWO_PI, YR, ALU.mult, ALU.add)
        ts(YR, YR, -math.pi, ALU.max, math.pi, ALU.min)
        act(tab, YR, AF.Sin)

    def rms_rstd(ss, n, rstd_out):
        ts(RTMP[:, 0:n], ss[:, 0:n], 1.0 / D, ALU.mult, EPS, ALU.add)
        act(RTMP2[:, 0:n], RTMP[:, 0:n], AF.Sqrt)
        recip(rstd_out[:, 0:n], RTMP2[:, 0:n])

    def make_hT(t, rstd_col, which, dstT, tloc):
        pb = bf(bank())
        for hf in range(2):
            xh = XN[:, hf * 512:(hf + 1) * 512]
            act(xh, X[:, t, hf * 512:(hf + 1) * 512], AF.Copy, scale=rstd_col)
            for k in range(4):
                tr(pb[:, (4 * hf + k) * 128:(4 * hf + k + 1) * 128], xh[:, k * 128:(k + 1) * 128])
        tt(dstT[:, :, tloc * 128:(tloc + 1) * 128], pb.rearrange("p (k c) -> p k c", c=128),
           GPRE[:, which, :].unsqueeze(2).to_broadcast([128, 8, 128]), ALU.mult)

    def post_norm_sq(t, pair):
        act(JUNK, pair, AF.Square, accum=SSP[:, t:t + 1])

    def post_norm_rstd(t):
        ts(PT1[:, t:t + 1], SSP[:, t:t + 1], 1.0 / D, ALU.mult, EPS, ALU.add)
        act(PT2[:, t:t + 1], PT1[:, t:t + 1], AF.Sqrt)
        recip(RSTDP[:, t:t + 1], PT2[:, t:t + 1])

    def post_norm_update(t, pair, idx):
        post_norm_sq(t, pair)
        post_norm_rstd(t)

    def post_norm_apply(t, pair, tmpbufs, next_ss=None, next_rstd=None, wide=None):
        if wide is not None:
            stt(wide, pair, RSTDP[:, t:t + 1], GPOST, ALU.mult, ALU.mult)
            tt(X[:, t, :], X[:, t, :], wide, ALU.add)
        else:
            for hf in range(2):
                tmp = tmpbufs[hf]
                stt(tmp, pair[:, hf * 512:(hf + 1) * 512], RSTDP[:, t:t + 1], GPOST[:, hf * 512:(hf + 1) * 512],
                    ALU.mult, ALU.mult)
                tt(X[:, t, hf * 512:(hf + 1) * 512], X[:, t, hf * 512:(hf + 1) * 512], tmp, ALU.add)
        if next_ss is not None:
            act(JUNK, X[:, t, :], AF.Square, accum=next_ss[:, t:t + 1])
        if next_rstd is not None:
            ts(RTMP[:, t:t + 1], next_ss[:, t:t + 1], 1.0 / D, ALU.mult, EPS, ALU.add)
            act(RTMP2[:, t:t + 1], RTMP[:, t:t + 1], AF.Sqrt)
            recip(next_rstd[:, t:t + 1], RTMP2[:, t:t + 1])

    def win_block(l, c0):
        s = next_slot()
        g = _Grp()
        sv = SLOT[s].rearrange("p (k c) -> p k c", c=512)
        dma("pool", sv, w_in[l, :, c0:c0 + 512].rearrange("(k p) c -> p k c", p=128), "ring%d" % s, g)
        return sv

    def stop_at(name):
        if stop == name:
            raise _Stop()

    def layer(l):
        gg = _Grp()
        dma("sp", GPRE[:, 0, :], g_mpre[l].rearrange("(k p) -> p k", p=128), "gpre", gg, noncontig=True)
        dma("sp", GPRE[:, 1, :], g_fpre[l].rearrange("(k p) -> p k", p=128), "gpre", gg, noncontig=True)

        if l == 0:
            for t in range(NT):
                act(JUNK, X[:, t, :], AF.Square, accum=SS[:, t:t + 1])
        rms_rstd(SS, NT, RSTD_MIX)
        for t in range(NT):
            make_hT(t, RSTD_MIX[:, t:t + 1], 0, HT, t)
        dump("hT%d" % l, HT, [8, S])

        stop_at('N1')
        memset(VA[:, :, :, 64:72], 0.0, q="pool")
        memset(VA[:, :, :, 64:65], 1.0, q="pool")
        svq = win_block(l, O_Q)
        svk = win_block(l, O_K)
        rope_state = {}

        def a_mm(t):
            pbs = []
            for sv in (svq, svk):
                pb = bank()
                for k in range(8):
                    mm(pb, HT[:, k, t * 128:(t + 1) * 128], sv[:, k, :], start=(k == 0), stop=(k == 7))
                pbs.append(pb)
            cosb = COS[:, t, :].unsqueeze(1).unsqueeze(1).to_broadcast([128, 8, 2, 32])
            sinb = SIN[:, t, :].unsqueeze(1).to_broadcast([128, 8, 32])
            p4 = [pb.rearrange("p (h two d) -> p h two d", two=2, d=32) for pb in pbs]
            t1 = [RT1[i].rearrange("p (h two d) -> p h two d", two=2, d=32) for i in range(2)]
            t2 = [RT2[i].rearrange("p (h two d) -> p h two d", two=2, d=32) for i in range(2)]
            for i in range(2):
                tt(t1[i], p4[i], cosb, ALU.mult)
            for i in range(2):
                stt(t2[i][:, :, 0, :], p4[i][:, :, 1, :], -1.0, sinb, ALU.mult, ALU.mult)
            for i in range(2):
                tt(t2[i][:, :, 1, :], p4[i][:, :, 0, :], sinb, ALU.mult)
            for i in range(2):
                tt(QR[2 * (t % 2) + i], RT1[i], RT2[i], ALU.add)

        def a_tr(t):
            for i in range(2):
                qr = QR[2 * (t % 2) + i]
                pt = bf(bank())
                for j in range(4):
                    tr(pt[:, j * 128:(j + 1) * 128], qr[:, j * 128:(j + 1) * 128])
                dst = (QT if i == 0 else KT)[:, :, t * 128:(t + 1) * 128]
                act(dst, pt[:, 0:512].rearrange("p (j c) -> p j c", c=128), AF.Copy,
                    scale=(0.125 if i == 0 else 1.0))

        for t in range(NT + 1):
            if t < NT:
                a_mm(t)
            if t >= 1:
                a_tr(t - 1)
        for pr in range(4):
            P.add("dve", (lambda pr=pr: nc.vector.tensor_reduce(
                out=KBAR[:, pr, :], in_=KT[:, pr, :].rearrange("p (n s) -> p n s", s=256), axis=AX.X, op=ALU.add)),
                reads=[KT[:, pr, :]], writes=[KBAR[:, pr, :]])
        cp(KBH, KBAR)
        tt(KBAR2, KBAR, KBH, ALU.subtract)
        cp(KBL, KBAR2)
        sv = win_block(l, O_VA)
        for t in range(NT):
            pb = bank()
            for k in range(8):
                mm(pb, HT[:, k, t * 128:(t + 1) * 128], sv[:, k, :], start=(k == 0), stop=(k == 7))
            act(VA[:, t, :, 0:64], pb.rearrange("p (h d) -> p h d", d=64), AF.Copy)
        dump("qT%d" % l, QT, [4, S])
        dump("kT%d" % l, KT, [4, S])
        dump("va%d" % l, VA, [NT, 8, 72])

        stop_at('A')

        stop_at('B0')

        def gates(b):
            for qi in range(2):
                qt = 2 * b + qi
                gbs = [bank(), bank()]
                for par in range(2):
                    lo = 64 * par
                    for pr in range(4):
                        o = gbs[par][:, pr * 8:(pr + 1) * 8]
                        mm(o, QT[lo:lo + 64, pr, qt * 128:(qt + 1) * 128], KBH[lo:lo + 64, pr, :], start=True, stop=False)
                        mm(o, QT[lo:lo + 64, pr, qt * 128:(qt + 1) * 128], KBL[lo:lo + 64, pr, :], start=False, stop=True)
                g4 = GSB.rearrange("p (hp two) n -> p hp two n", two=2)
                for par in range(2):
                    cp(g4[:, :, par, :], gbs[par][:, 0:32].rearrange("p (h n) -> p h n", n=8))
                memset(GSB[:, :, b:8], -1e30)
                for h in range(8):
                    P.add("dve", (lambda h=h: nc.vector.max(out=TOP8[:, h, :], in_=GSB[:, h, :])),
                          reads=[GSB[:, h, :]], writes=[TOP8[:, h, :]])
                tt(CMP, GSB, TOP8[:, :, 2:3].to_broadcast([128, 8, 8]), ALU.is_lt)
                ts(MBTOK.rearrange("p (r c) -> p r c", c=64),
                   CMP.rearrange("p h n -> p (h n)").unsqueeze(1).to_broadcast([128, 2, 64]), NEG, ALU.mult)
                pm = bf(bank())
                tr(pm[:, 0:128], MBTOK)
                cp(MBT[:, (qt - 8) * 128:(qt - 8 + 1) * 128], pm[:, 0:128])

        def score_tiles(b, pr, ub):
            PT = PTB[ub]
            tiles = []

            def past_mm(m, st):
                sbs = [bank(), bank()]
                st["sbs"] = sbs
                for j in range(2):
                    kt = 2 * m + j
                    for par in range(2):
                        lo = 64 * par
                        mm(sbs[par][:, j * 256:(j + 1) * 256], KT[lo:lo + 64, pr, kt * 128:(kt + 1) * 128],
                           QT[lo:lo + 64, pr, b * 256:(b + 1) * 256], start=True, stop=(b < 4))
                    if b >= 4:
                        for par in range(2):
                            lo = 64 * par
                            h = 2 * pr + par
                            sel = IDENT[lo:lo + 64, lo + h * 8 + m:lo + h * 8 + m + 1].to_broadcast([64, 128])
                            mm(sbs[par][:, j * 256:(j + 1) * 256], sel,
                               MBT[lo:lo + 64, (b - 4) * 256:(b - 4 + 1) * 256], start=False, stop=True)

            def past_exp(m, st):
                sbs = st["sbs"]
                for par in range(2):
                    act(PT[par][:, 2 * m:2 * m + 2, :], sbs[par].rearrange("p (j c) -> p j c", c=256), AF.Exp)

            def own_mm(st):
                sbs = [bank(), bank()]
                st["sbs"] = sbs
                kt = 2 * b
                for par in range(2):
                    lo = 64 * par
                    mm(sbs[par][:, 0:128], KT[lo:lo + 64, pr, kt * 128:(kt + 1) * 128],
                       QT[lo:lo + 64, pr, b * 256:b * 256 + 128], start=True, stop=False)
                for par in range(2):
                    mm(sbs[par][:, 0:128], IDENT, TRI, start=False, stop=True)
                for par in range(2):
                    lo = 64 * par
                    mm(sbs[par][:, 128:256], KT[lo:lo + 64, pr, kt * 128:(kt + 1) * 128],
                       QT[lo:lo + 64, pr, b * 256 + 128:b * 256 + 256], start=True, stop=True)
                kt = 2 * b + 1
                for par in range(2):
                    lo = 64 * par
                    mm(sbs[par][:, 256:384], KT[lo:lo + 64, pr, kt * 128:(kt + 1) * 128],
                       QT[lo:lo + 64, pr, b * 256 + 128:b * 256 + 256], start=True, stop=False)
                for par in range(2):
                    mm(sbs[par][:, 256:384], IDENT, TRI, start=False, stop=True)

            def own_exp(st):
                sbs = st["sbs"]
                for par in range(2):
                    act(PT[par][:, 2 * b, :], sbs[par][:, 0:256], AF.Exp)
                    act(PT[par][:, 2 * b + 1, 128:256], sbs[par][:, 256:384], AF.Exp)

            for m in range(b):
                st = {}
                tiles.append(((lambda m=m, st=st: past_mm(m, st)), (lambda m=m, st=st: past_exp(m, st))))
            st = {}
            tiles.append(((lambda st=st: own_mm(st)), (lambda st=st: own_exp(st))))
            return tiles

        def pv(b, h, PTv, ybuf):
            ab = bank()
            for qi in range(2):
                nk = 2 * b + qi + 1
                for kt in range(nk):
                    mm(ab[:, qi * 128:qi * 128 + 66], PTv[:, kt, qi * 128:(qi + 1) * 128], VA[:, kt, h, 0:66],
                       start=(kt == 0), stop=(kt == nk - 1))
            a3 = ab[:, 0:256].rearrange("p (q c) -> p q c", c=128)
            rc = REC[h % 2]
            recip(rc, a3[:, :, 64])
            tt(YBTOK[ybuf][:, :, h * 64:(h + 1) * 64], a3[:, :, 0:64], rc.unsqueeze(2).to_broadcast([128, 2, 64]),
               ALU.mult)

        def yb_transpose(b, ybuf):
            pt = bf(bank())
            p3 = pt.rearrange("p (c q) -> p c q", q=256)
            for qi in range(2):
                for c in range(4):
                    tr(p3[:, c, qi * 128:(qi + 1) * 128], YBTOK[ybuf][:, qi, c * 128:(c + 1) * 128])
            act(YBT[:, :, b * 256:(b + 1) * 256], p3, AF.Copy)

        units = [(b, pr) for b in range(8) for pr in range(4)]

        def pv_unit(i):
            pb_, ppr = units[i]
            for par in range(2):
                pv(pb_, 2 * ppr + par, PTB[i % 2][par], pb_ % 2)
            if ppr == 3:
                yb_transpose(pb_, pb_ % 2)

        for i, (b, pr) in enumerate(units):
            if pr == 0 and b >= 4:
                gates(b)
            for fm, fe in score_tiles(b, pr, i % 2):
                fm()
                fe()
            if i >= 1:
                pv_unit(i - 1)
        pv_unit(len(units) - 1)
        dump("ybT%d" % l, YBT, [4, S])
        dump("mbt%d" % l, MBT, [1024])

        stop_at('B')
        for t in range(NT):
            make_hT(t, RSTD_MIX[:, t:t + 1], 0, HT, t)

        stop_at('N2')
        gc = _Grp()
        dma("sp", LNGc, ln_g[l].rearrange("(gp p) -> p gp", p=128), "lng", gc, noncontig=True)
        gc2 = _Grp()
        dma("sp", LNBc, ln_b[l].rearrange("(gp p) -> p gp", p=128), "lnb", gc2, noncontig=True)
        gc3 = _Grp()
        for g in range(8):
            dma("sp", BSB[64 * (g % 2):64 * (g % 2) + 64, g // 2, :], b_s[l, g].partition_broadcast(64), "bsb", gc3)
        gc4 = _Grp()
        dma("sp", WSS, w_s[l].rearrange("g i j -> i g j"), "wss", gc4)
        cp(WS16, WSS)
        pw = bf(bank())
        for g in range(8):
            tr(pw[:, g * 128:(g + 1) * 128], WS16[:, g, :])
        tt(WST, pw.rearrange("p (g c) -> p g c", c=128), MASK01.unsqueeze(1).to_broadcast([128, 8, 128]), ALU.mult)
        prs = bank()
        for gp in range(4):
            for e in range(2):
                mm(prs[64 * e:64 * e + 64, gp * 128:(gp + 1) * 128], ONES16[:, 0:64], WST[:, 2 * gp + e, :])
        for gp in range(4):
            stt(BIAS2[:, gp, :], prs[:, gp * 128:(gp + 1) * 128], LNBc[:, gp:gp + 1], BSB[:, gp, :], ALU.mult, ALU.add)

        sv = win_block(l, O_U)
        for m in range(4):
            for tg in range(4):
                pb = bank()
                for k in range(8):
                    mm(pb, sv[:, k, m * 128:(m + 1) * 128], HT[:, k, tg * 512:(tg + 1) * 512], start=(k == 0), stop=(k == 7))
                act(UT[:, m, tg * 512:(tg + 1) * 512], pb, AF.Gelu)
        sv = win_block(l, O_VG)

        def vg_stage1(g):
            for t in range(4 * g, 4 * g + 4):
                i = t % 8
                pb = bank()
                for k in range(8):
                    mm(pb, HT[:, k, t * 128:(t + 1) * 128], sv[:, k, :], start=(k == 0), stop=(k == 7))
                act(G32[i], pb, AF.Gelu)
            for t in range(4 * g, 4 * g + 4):
                i = t % 8
                P.add("dve", (lambda i=i: nc.vector.bn_stats(out=BNST[i], in_=G32[i])), reads=[G32[i]], writes=[BNST[i]])
            for t in range(4 * g, 4 * g + 4):
                i = t % 8
                P.add("dve", (lambda i=i: nc.vector.bn_aggr(out=MV8[:, i, :], in_=BNST[i])), reads=[BNST[i]],
                      writes=[MV8[:, i, :]])

        def vg_stage23(g):
            i0 = (4 * g) % 8
            ts(LNT4[:, i0:i0 + 4], MV8[:, i0:i0 + 4, 1], EPS, ALU.add)
            act(LNT24[:, i0:i0 + 4], LNT4[:, i0:i0 + 4], AF.Sqrt)
            recip(LNR4[:, i0:i0 + 4], LNT24[:, i0:i0 + 4])
            tl = list(range(4 * g, 4 * g + 4))
            for t in tl:
                i = t % 8
                ts(V16[t % 4], G32[i], MV8[:, i, 0:1], ALU.subtract, LNR4[:, i:i + 1], ALU.mult)
            pbs = {}
            for t in tl:
                pb = bank()
                pbs[t] = pb
                for gp in range(4):
                    for e in range(2):
                        gg_ = 2 * gp + e
                        mm(pb[64 * e:64 * e + 64, gp * 128:(gp + 1) * 128], V16[t % 4][:, gg_ * 64:(gg_ + 1) * 64],
                           WST[:, gg_, :])
            for t in tl:
                g3 = G32[t % 8].rearrange("p (a b) -> p a b", b=128)
                for gp in range(4):
                    stt(g3[:, gp, :], pbs[t][:, gp * 128:(gp + 1) * 128], LNGc[:, gp:gp + 1], BIAS2[:, gp, :],
                        ALU.mult, ALU.add)
            for t in tl:
                ya = UT[:, :, t * 128:(t + 1) * 128]
                tt(ya, G32[t % 8].rearrange("p (a b) -> p a b", b=128), ya, ALU.mult)

        for g in range(5):
            if g < 4:
                vg_stage1(g)
            if g >= 1:
                vg_stage23(g - 1)
        dump("yaT%d" % l, UT, [4, S])

        stop_at('C1')
        for m in range(8):
            s = next_slot()
            g = _Grp()
            sl = SLOT[s]
            GAv = sl[:, 0:1024].rearrange("p (k c) -> p k c", c=128)
            GBv = sl[:, 1024:2048].rearrange("p (k c) -> p k c", c=128)
            WAv = sl[:, 2048:2560].rearrange("p (k c) -> p k c", c=128)
            WBv = sl[:, 2560:3072].rearrange("p (k c) -> p k c", c=128)
            sem = "ring%d" % s
            dma("pool", GAv, w_in[l, :, O_GA + m * 128:O_GA + (m + 1) * 128].rearrange("(k p) c -> p k c", p=128), sem, g)
            dma("pool", GBv, w_in[l, :, O_GB + m * 128:O_GB + (m + 1) * 128].rearrange("(k p) c -> p k c", p=128), sem, g)
            dma("pool", WAv, w_pa[l, :, m * 128:(m + 1) * 128].rearrange("(k p) c -> p k c", p=128), sem, g)
            dma("pool", WBv, w_pb[l, :, m * 128:(m + 1) * 128].rearrange("(k p) c -> p k c", p=128), sem, g)
            for tg in range(4):
                tok = slice(tg * 512, (tg + 1) * 512)
                bA, bB, bC, bD = bank(), bank(), bank(), bank()
                for k in range(8):
                    mm(bB, GAv[:, k, :], HT[:, k, tok], start=(k == 0), stop=(k == 7))
                for k in range(4):
                    mm(bA, WAv[:, k, :], UT[:, k, tok], start=(k == 0), stop=(k == 3))
                for k in range(8):
                    mm(bD, GBv[:, k, :], HT[:, k, tok], start=(k == 0), stop=(k == 7))
                for k in range(4):
                    mm(bC, WBv[:, k, :], YBT[:, k, tok], start=(k == 0), stop=(k == 3))
                act(SGA, bB, AF.Sigmoid)
                act(SGB, bD, AF.Sigmoid)
                tt(T1, bA, SGA, ALU.mult)
                tt(T2, bC, SGB, ALU.mult)
                tt(MT[:, m, tok], T1, T2, ALU.add)
        dump("mT%d" % l, MT, [8, S])

        stop_at('C2')
        gp_ = _Grp()
        dma("sp", GPOST, g_mpost[l].partition_broadcast(128), "gpost", gp_)
        wo = []
        for nb in range(2):
            s = next_slot()
            g = _Grp()
            svo = SLOT[s].rearrange("p (k c) -> p k c", c=512)
            dma("pool", svo, w_out[l, :, nb * 512:(nb + 1) * 512].rearrange("(k p) c -> p k c", p=128), "ring%d" % s, g)
            wo.append(svo)
        pairs = {}
        for t in range(NT + 2):
            if t < NT:
                pr_ = bank_pair(keep=3)
                pairs[t] = pr_
                for nb in range(2):
                    for k in range(8):
                        mm(pr_[:, nb * 512:(nb + 1) * 512], MT[:, k, t * 128:(t + 1) * 128], wo[nb][:, k, :],
                           start=(k == 0), stop=(k == 7))
                post_norm_sq(t, pr_)
            if 1 <= t <= NT:
                post_norm_rstd(t - 1)
            if t >= 2:
                post_norm_apply(t - 2, pairs[t - 2], TMPC, next_ss=SSF, wide=TMPCW[t % 2])
        live_pair[0] = None
        rms_rstd(SSF, NT, RSTD_F)
        for tl in range(8):
            make_hT(tl, RSTD_F[:, tl:tl + 1], 1, HTH, tl)
        live_pair[0] = None
        dump("xmid%d" % l, X, [NT, D])

        stop_at('C3')
        for hf in range(2):
            for ffp in range(NFF // 2):
                s = next_slot()
                g = _Grp()
                sv = SLOT[s].rearrange("p (k c) -> p k c", c=512)
                sem = "ring%d" % s
                dma("pool", sv[:, :, 0:256], w_gu[l, :, ffp * 256:(ffp + 1) * 256].rearrange("(k p) c -> p k c", p=128), sem, g)
                dma("pool", sv[:, :, 256:512],
                    w_gu[l, :, DFF + ffp * 256:DFF + (ffp + 1) * 256].rearrange("(k p) c -> p k c", p=128), sem, g)
                if hf == 0 and ffp == 2:
                    gw = _Grp()
                    for (k0, k1) in ((0, 6), (6, 12), (12, 18), (18, 22)):
                        dma("pool", WD[:, k0:k1, :], w_dn[l, k0 * 128:k1 * 128, :].rearrange("(k p) c -> p k c", p=128), "wd", gw)
                    gq = _Grp()
                    dma("sp", GPOST, g_fpost[l].partition_broadcast(128), "gpost", gq)
                for c in range(2):
                    ffc = 2 * ffp + c
                    for tg in range(2):
                        tok = slice(tg * 512, (tg + 1) * 512)
                        bG, bU = bank(), bank()
                        for k in range(8):
                            mm(bG, sv[:, k, c * 128:(c + 1) * 128], HTH[:, k, tok], start=(k == 0), stop=(k == 7))
                        for k in range(8):
                            mm(bU, sv[:, k, 256 + c * 128:256 + (c + 1) * 128], HTH[:, k, tok], start=(k == 0), stop=(k == 7))
                        si = (ffc * 2 + tg) % 2
                        act(SL[si], bG, AF.Silu)
                        tt(ACTT[:, ffc, tok], bU, SL[si], ALU.mult)
            if hf == 0:
                for tl in range(8):
                    make_hT(8 + tl, RSTD_F[:, 8 + tl:8 + tl + 1], 1, HTH, tl)
            pairs = {}
            for tl in range(9):
                if tl < 8:
                    t = 8 * hf + tl
                    pr_ = bank_pair()
                    pairs[tl] = pr_
                    for nb in range(2):
                        for k in range(NFF):
                            mm(pr_[:, nb * 512:(nb + 1) * 512], ACTT[:, k, tl * 128:(tl + 1) * 128],
                               WD[:, k, nb * 512:(nb + 1) * 512], start=(k == 0), stop=(k == NFF - 1))
                    post_norm_update(t, pr_, t)
                if tl >= 1:
                    post_norm_apply(8 * hf + tl - 1, pairs[tl - 1], TMPF, next_ss=(SS if l + 1 < L else None))
            live_pair[0] = None
        dump("xout%d" % l, X, [NT, D])


    try:
        stop_at('setup')
        for l in range(L):
            layer(l)
    except _Stop:
        pass

    go = _Grp()
    for t in range(NT):
        dma("sp", out[t * 128:(t + 1) * 128, :], X[:, t, :], "ost", go)
    P.add("sp", lambda: nc.sync.nop() if hasattr(nc.sync, "nop") else nc.sync.engine_nop(), reads=[], writes=[])
    fin = P.ops[-1]
    fin.waits = [go.ops[-1]]
    nsem = P.emit()
    return nc, dbg_outs, len(P.ops), nsem


_CACHE = {}


def _get_prog(nlayers):
    if nlayers not in _CACHE:
        _CACHE[nlayers] = build_program(nlayers)
    return _CACHE[nlayers]


W_NAMES = ["w_in", "w_s", "b_s", "ln_v_g", "ln_v_b", "w_proj_a", "w_proj_b", "w_out", "g_mix_pre", "g_mix_post",
           "g_ffn_pre", "g_ffn_post", "w_gate_up", "w_down"]


def kernel(x, positions, **w):
    x = np.asarray(x, dtype=np.float32)
    positions = np.asarray(positions, dtype=np.int32)
    ws = {k: np.ascontiguousarray(np.asarray(w[k], dtype=np.float32)) for k in W_NAMES}
    nc, _, _, _ = _get_prog(DEPTH)
    in_maps = []
    for c in range(NCORES):
        m = {"x": np.ascontiguousarray(x[c]), "positions": np.ascontiguousarray(positions[c])}
        m.update(ws)
        in_maps.append(m)
    res = run_bass_kernel_spmd(nc, in_maps, core_ids=list(range(NCORES)))
    return np.stack([np.asarray(res.results[c]["out"], dtype=np.float32) for c in range(NCORES)], axis=0)
```

```python
import itertools
import math
import numpy as np
import concourse.bass as bass
import concourse.mybir as mybir
from concourse.bass_utils import run_bass_kernel_spmd

F32 = mybir.dt.float32
BF16 = mybir.dt.bfloat16
U8 = mybir.dt.uint8
I32 = mybir.dt.int32
AF = mybir.ActivationFunctionType
ALU = mybir.AluOpType
AX = mybir.AxisListType

D = 1024
S = 2048
NT = S // 128
DEPTH = 4
NCORES = 8
GMW = 512
AW = 512
DFF = 2816
NFF = DFF // 128
INW = 2 * GMW + 3 * AW + 2 * D
O_U, O_VG, O_Q, O_K, O_VA, O_GA, O_GB = 0, 512, 1024, 1536, 2048, 2560, 3584
EPS = 1e-6
NEG = -30000.0
TWO_PI = 2.0 * math.pi


def _dsize(dt):
    return mybir.dt.size(dt)


class _Op:
    __slots__ = ("idx", "q", "stream", "fn", "waits", "signal", "clock", "sigval", "grp", "isdma")


class _Grp:
    def __init__(self):
        self.ops = []


class Prog:
    def __init__(self, nc):
        self.nc = nc
        self.ops = []
        self.lastw = {}
        self.readers = {}
        self.known = {}
        self.gran = {}
        self.engs = {"pe": nc.tensor, "act": nc.scalar, "dve": nc.vector, "pool": nc.gpsimd, "sp": nc.sync}
        self._fcache = {}

    def chunks(self, ap):
        name = ap.tensor.name
        g = self.gran.get(name)
        if g is None:
            return ()
        pairs = tuple(ap.ap)
        key = (name, ap.offset, pairs, ap.dtype)
        r = self._fcache.get(key)
        if r is not None:
            return r
        es = _dsize(ap.dtype)
        rowstride = pairs[0][0]
        off = ap.offset % rowstride
        free = pairs[1:]
        if not free:
            free = ((1, 1),)
        outer = free[:-1]
        lstep, lcnt = free[-1]
        llen = ((lcnt - 1) * lstep + 1) if lstep != 0 else 1
        res = set()
        for idxs in itertools.product(*[range(c) for (_, c) in outer]):
            st = off + sum(s * i for (s, _), i in zip(outer, idxs))
            b0 = st * es
            b1 = (st + llen) * es - 1
            for c in range(b0 // g, b1 // g + 1):
                res.add((name, c))
        r = tuple(res)
        self._fcache[key] = r
        return r

    def add(self, q, fn, reads=(), writes=(), dsem=None, grp=None):
        op = _Op()
        op.idx = len(self.ops)
        op.q = q
        op.isdma = dsem is not None
        op.stream = ("D:" + dsem) if dsem is not None else q
        op.fn = fn
        op.signal = False
        op.sigval = None
        op.grp = grp
        if grp is not None:
            grp.ops.append(op)
        rch = set()
        for a in reads:
            rch.update(self.chunks(a))
        wch = set()
        for a in writes:
            wch.update(self.chunks(a))
        deps = {}

        def need(i):
            t = self.ops[i]
            if t.grp is not None:
                t = t.grp.ops[-1]
            s = t.stream
            if deps.get(s, -1) < t.idx:
                deps[s] = t.idx

        for c in rch:
            w = self.lastw.get(c)
            if w is not None:
                need(w)
        for c in wch:
            w = self.lastw.get(c)
            if w is not None:
                need(w)
            rd = self.readers.get(c)
            if rd:
                for i in rd.values():
                    need(i)
        kn = self.known.setdefault(q, {})
        waits = []
        for s, i in deps.items():
            if s == "pe" and q == "pe" and not op.isdma:
                continue
            if i == op.idx:
                continue
            if kn.get(s, -1) >= i:
                continue
            t = self.ops[i]
            t.signal = True
            waits.append(t)
            for s2, i2 in t.clock.items():
                if kn.get(s2, -1) < i2:
                    kn[s2] = i2
        op.waits = waits
        op.clock = dict(kn)
        op.clock[op.stream] = op.idx
        for c in rch:
            self.readers.setdefault(c, {})[op.stream] = op.idx
        for c in wch:
            self.lastw[c] = op.idx
            self.readers[c] = {}
        self.ops.append(op)
        return op

    def emit(self):
        nc = self.nc
        handles = {}
        counts = {}

        def H(s):
            if s not in handles:
                handles[s] = nc.alloc_semaphore("s_" + s.replace(":", "_"))
                counts[s] = 0
            return handles[s]

        for op in self.ops:
            eng = self.engs[op.q]
            wl = {}
            for t in op.waits:
                assert t.sigval is not None, (op.idx, t.idx, t.stream)
                if wl.get(t.stream, -1) < t.sigval:
                    wl[t.stream] = t.sigval
            wl = list(wl.items())
            for s, v in wl[:-1]:
                eng.wait_ge(H(s), v)
            ins = op.fn()
            if wl:
                s, v = wl[-1]
                ins._wait_ge(H(s), v)
            if op.isdma:
                h = H(op.stream)
                counts[op.stream] += 16
                ins.then_inc(h, 16)
                op.sigval = counts[op.stream]
            elif op.signal:
                h = H(op.stream)
                counts[op.stream] += 1
                ins.then_inc(h, 1)
                op.sigval = counts[op.stream]
        return len(handles)


class _Stop(Exception):
    pass


def build_program(nlayers, dbg=None, stop=None):
    nc = bass.Bass("TRN2", target_bir_lowering=False)
    L = nlayers

    def din(name, shape, dt=F32):
        return nc.dram_tensor(name, shape, dt, kind="ExternalInput").ap()

    x_in = din("x", [S, D])
    pos_in = din("positions", [S], I32)
    w_in = din("w_in", [L, D, INW])
    w_s = din("w_s", [L, 8, 128, 128])
    b_s = din("b_s", [L, 8, 128])
    ln_g = din("ln_v_g", [L, GMW])
    ln_b = din("ln_v_b", [L, GMW])
    w_pa = din("w_proj_a", [L, GMW, D])
    w_pb = din("w_proj_b", [L, AW, D])
    w_out = din("w_out", [L, D, D])
    g_mpre = din("g_mix_pre", [L, D])
    g_mpost = din("g_mix_post", [L, D])
    g_fpre = din("g_ffn_pre", [L, D])
    g_fpost = din("g_ffn_post", [L, D])
    w_gu = din("w_gate_up", [L, D, 2 * DFF])
    w_dn = din("w_down", [L, DFF, D])
    out = nc.dram_tensor("out", [S, D], F32, kind="ExternalOutput").ap()
    dbg_outs = {}

    P = Prog(nc)

    XB = 65536
    BIGB = 106496
    RINGB = 3 * 8192
    SMB = 16128
    Xa = nc.alloc_sbuf_tensor("X", [128, XB], U8)[:]
    Ba = nc.alloc_sbuf_tensor("B", [128, BIGB], U8)[:]
    Ra = nc.alloc_sbuf_tensor("R", [128, RINGB], U8)[:]
    Sa = nc.alloc_sbuf_tensor("SM", [128, SMB], U8)[:]
    PSa = nc.alloc_psum_tensor("PS", [128, 4096], F32)[:]
    P.gran = {"X": 2048, "B": 256, "R": 512, "SM": 32, "PS": 2048}

    def view(arena, off, dt, shape):
        n = int(np.prod(shape)) * _dsize(dt)
        v = arena[:, off:off + n].bitcast(dt)
        if len(shape) == 2:
            v = v.rearrange("p (a b) -> p a b", b=shape[1])
        elif len(shape) == 3:
            v = v.rearrange("p (a b c) -> p a b c", b=shape[1], c=shape[2])
        return v

    X = view(Xa, 0, F32, [NT, D])

    QT = view(Ba, 0, BF16, [4, S])
    KT = view(Ba, 16384, BF16, [4, S])
    VA = view(Ba, 32768, BF16, [NT, 8, 72])
    R1 = 51200
    HT = view(Ba, R1, BF16, [8, S])
    PTB = [[view(Ba, R1 + (u * 2 + p_) * 8192, BF16, [16, 256]) for p_ in range(2)] for u in range(2)]
    R3 = R1 + 32768
    YBT = view(Ba, R3, BF16, [4, S])
    R4 = R3 + 16384
    MBT = view(Ba, R4, BF16, [1024])
    YBTOK = [view(Ba, R4 + 2048, BF16, [2, 512]), view(Ba, R4 + 4096, BF16, [2, 512])]
    RT1 = [view(Ba, R3, F32, [512]), view(Ba, R3 + 2048, F32, [512])]
    RT2 = [view(Ba, R3 + 4096, F32, [512]), view(Ba, R3 + 6144, F32, [512])]
    QR = [view(Ba, R3 + 8192 + 1024 * i, BF16, [512]) for i in range(4)]
    BSB = view(Ba, 0, F32, [4, 128])
    BIAS2 = view(Ba, 2048, F32, [4, 128])
    WST = view(Ba, 4096, BF16, [8, 128])
    WSS = view(Ba, 8192, F32, [8, 128])
    WS16 = view(Ba, 12288, BF16, [8, 128])
    G32 = [view(Ba, 8192 + 2048 * i, F32, [512]) for i in range(8)]
    V16 = [view(Ba, 24576 + 1024 * i, BF16, [512]) for i in range(4)]
    UT = view(Ba, 32768, BF16, [4, S])
    MT = view(Ba, 0, BF16, [8, S])
    SGA = view(Ba, R4, BF16, [512])
    SGB = view(Ba, R4 + 1024, BF16, [512])
    T1 = view(Ba, R4 + 2048, F32, [512])
    T2 = view(Ba, R4 + 4096, F32, [512])
    TMPC = [view(Ba, R3, F32, [512]), view(Ba, R3 + 2048, F32, [512])]
    TMPCW = [view(Ba, R3, F32, [1024]), view(Ba, R3 + 4096, F32, [1024])]
    ACTT = view(Ba, 0, BF16, [NFF, 1024])
    HTH = view(Ba, 45056, BF16, [8, 1024])
    WD = view(Ba, 61440, BF16, [NFF, 1024])
    TMPF = None
    SU = 0

    SLOT = [view(Ra, i * 8192, BF16, [4096]) for i in range(3)]
    ring_n = [0]

    def next_slot():
        s = ring_n[0] % 3
        ring_n[0] += 1
        return s

    so = [0]
    soff = {}

    def salloc(dt, shape, align=32, name=None):
        n = int(np.prod(shape)) * _dsize(dt)
        so[0] = (so[0] + align - 1) // align * align
        v = view(Sa, so[0], dt, shape)
        if name is not None:
            soff[name] = so[0]
        so[0] += n
        return v

    GPOST = salloc(F32, [D])
    XN = salloc(BF16, [D])
    TMPF = [XN.bitcast(F32), XN.bitcast(F32)]
    COS = salloc(F32, [NT, 32])
    SIN = salloc(F32, [NT, 32])
    SL = [salloc(BF16, [512], name="SL0"), salloc(BF16, [512])]
    IDENT = salloc(BF16, [128])
    TRI = salloc(BF16, [128])
    MASK01 = salloc(BF16, [128])
    GPRE = salloc(F32, [2, 8])
    SS = salloc(F32, [NT])
    SSF = salloc(F32, [NT])
    RTMP = salloc(F32, [NT])
    RTMP2 = salloc(F32, [NT])
    RSTD_MIX = salloc(F32, [NT])
    RSTD_F = salloc(F32, [NT])
    SSP = salloc(F32, [NT])
    PT1 = salloc(F32, [NT])
    PT2 = salloc(F32, [NT])
    RSTDP = salloc(F32, [NT])
    BNST = [salloc(F32, [6]) for _ in range(8)]
    MV8 = salloc(F32, [8, 2])
    LNT4 = salloc(F32, [8])
    LNT24 = salloc(F32, [8])
    LNR4 = salloc(F32, [8])
    LNGc = salloc(F32, [4])
    LNBc = salloc(F32, [4])
    ONES16 = salloc(BF16, [64])
    GSB = salloc(F32, [8, 8])
    TOP8 = salloc(F32, [8, 8])
    CMP = salloc(F32, [8, 8])
    MBTOK = salloc(BF16, [128])
    KBAR = salloc(F32, [4, 8])
    KBAR2 = salloc(F32, [4, 8])
    KBH = salloc(BF16, [4, 8])
    KBL = salloc(BF16, [4, 8])
    REC = [salloc(F32, [2]), salloc(F32, [2])]
    assert so[0] <= SMB, so[0]
    JUNK = view(Sa, soff["SL0"], BF16, [1024])

    bank_n = [0]
    pair_n = [0]

    live_pair = [None]

    def bank():
        while True:
            b = bank_n[0] % 8
            bank_n[0] += 1
            if live_pair[0] is None or (b // 2) not in live_pair[0]:
                break
        return PSa[:, b * 512:(b + 1) * 512]

    def bank_pair(keep=2):
        b = pair_n[0] % 4
        pair_n[0] += 1
        lp = (live_pair[0] or []) + [b]
        live_pair[0] = lp[-keep:]
        return PSa[:, b * 1024:(b + 1) * 1024]

    def bf(ps):
        return ps.bitcast(BF16)

    def mm(out_, lhsT, rhs, start=True, stop=True):
        P.add("pe", lambda: nc.tensor.matmul(out_, lhsT=lhsT, rhs=rhs, start=start, stop=stop),
              reads=[lhsT, rhs], writes=[out_])

    def tr(out_, in_):
        idn = IDENT[0:in_.partition_size(), 0:in_.partition_size()]
        P.add("pe", lambda: nc.tensor.transpose(out_, in_, idn), reads=[in_, idn], writes=[out_])

    def act(out_, in_, func, scale=None, bias=None, accum=None, q="act"):
        reads = [in_]
        kw = {}
        if scale is not None:
            kw["scale"] = scale
            if not isinstance(scale, (int, float)):
                reads.append(scale)
        if bias is not None:
            kw["bias"] = bias
            if not isinstance(bias, (int, float)):
                reads.append(bias)
        writes = [out_]
        if accum is not None:
            kw["accum_out"] = accum
            writes.append(accum)
        P.add(q, lambda: nc.scalar.activation(out=out_, in_=in_, func=func, **kw), reads=reads, writes=writes)

    def E(q):
        return P.engs[q]

    def tt(out_, in0, in1, op, q="dve"):
        P.add(q, lambda: E(q).tensor_tensor(out=out_, in0=in0, in1=in1, op=op), reads=[in0, in1], writes=[out_])

    def ts(out_, in0, s1, op0, s2=None, op1=None, q="dve"):
        reads = [in0]
        if not isinstance(s1, (int, float)):
            reads.append(s1)
        if s2 is not None and not isinstance(s2, (int, float)):
            reads.append(s2)
        if op1 is None:
            P.add(q, lambda: E(q).tensor_scalar(out=out_, in0=in0, scalar1=s1, scalar2=None, op0=op0),
                  reads=reads, writes=[out_])
        else:
            P.add(q, lambda: E(q).tensor_scalar(out=out_, in0=in0, scalar1=s1, scalar2=s2, op0=op0, op1=op1),
                  reads=reads, writes=[out_])

    def stt(out_, in0, scalar, in1, op0, op1, q="dve"):
        reads = [in0, in1]
        if not isinstance(scalar, (int, float)):
            reads.append(scalar)
        P.add(q, lambda: E(q).scalar_tensor_tensor(out=out_, in0=in0, scalar=scalar, in1=in1, op0=op0, op1=op1),
              reads=reads, writes=[out_])

    def cp(out_, in_, q="dve"):
        P.add(q, lambda: E(q).tensor_copy(out=out_, in_=in_), reads=[in_], writes=[out_])

    def memset(out_, val, q="dve"):
        P.add(q, lambda: E(q).memset(out_, val), writes=[out_])

    def recip(out_, in_):
        P.add("dve", lambda: nc.vector.reciprocal(out=out_, in_=in_), reads=[in_], writes=[out_])

    def dma(q, out_, in_, sem, grp, noncontig=False):
        if noncontig:
            fn = lambda: E(q).dma_start(out=out_, in_=in_, allow_slow_non_contiguous=True)
        else:
            fn = lambda: E(q).dma_start(out=out_, in_=in_)
        P.add(q, fn, reads=[in_], writes=[out_], dsem=sem, grp=grp)

    def dump(name, v, shape):
        if dbg is None or name not in dbg:
            return
        o = nc.dram_tensor("dbg_" + name, [128] + list(shape), v.dtype, kind="ExternalOutput").ap()
        dbg_outs[name] = o
        g = _Grp()
        dma("sp", o, v, "dbg_" + name, g)

    TF = view(Ba, SU, F32, [128])
    memset(TF, 0.0, q="pool")
    P.add("pool", lambda: nc.gpsimd.affine_select(out=TF, in_=TF, pattern=[[-1, 128]], compare_op=ALU.not_equal,
                                                    fill=1.0, base=0, channel_multiplier=1), reads=[TF], writes=[TF])
    cp(IDENT, TF)
    TF2 = view(Ba, SU + 512, F32, [128])
    memset(TF2, 0.0, q="pool")
    P.add("pool", lambda: nc.gpsimd.affine_select(out=TF2, in_=TF2, pattern=[[1, 128]], compare_op=ALU.is_ge,
                                                    fill=NEG, base=0, channel_multiplier=-1), reads=[TF2], writes=[TF2])
    cp(TRI, TF2)
    TF3 = view(Ba, SU + 1024, F32, [128])
    memset(TF3, 1.0, q="pool")
    P.add("pool", lambda: nc.gpsimd.affine_select(out=TF3, in_=TF3, pattern=[[1, 128]], compare_op=ALU.is_ge,
                                                    fill=0.0, base=0, channel_multiplier=-1), reads=[TF3], writes=[TF3])
    cp(MASK01, TF3)
    memset(ONES16, 1.0)

    PI_ = view(Ba, SU + 2048, I32, [NT])
    PF = view(Ba, SU + 2048 + 64, F32, [NT])
    gpos = _Grp()
    for t in range(NT):
        dma("sp", PI_[:, t:t + 1], pos_in[t * 128:(t + 1) * 128].rearrange("(p o) -> p o", o=1), "pos", gpos)
    gx = [_Grp() for _ in range(8)]
    for t in range(NT):
        dma("sp", X[:, t, :], x_in[t * 128:(t + 1) * 128, :], "xld%d" % (t // 2), gx[t // 2])
    cp(PF, PI_)
    IO = view(Ba, SU + 2304, F32, [32])
    P.add("pool", lambda: nc.gpsimd.iota(IO, pattern=[[1, 32]], base=0, channel_multiplier=0,
                                          allow_small_or_imprecise_dtypes=True), writes=[IO])
    INVF = view(Ba, SU + 2560, F32, [32])
    act(INVF, IO, AF.Exp, scale=-math.log(10000.0) / 32.0)
    ANG = view(Ba, SU + 4096, F32, [NT, 32])
    tt(ANG, PF.unsqueeze(2).to_broadcast([128, NT, 32]), INVF.unsqueeze(1).to_broadcast([128, NT, 32]), ALU.mult)
    A2 = view(Ba, SU + 6144, F32, [NT, 32])
    KF = view(Ba, SU + 8192, F32, [NT, 32])
    KI = view(Ba, SU + 10240, I32, [NT, 32])
    YR = view(Ba, SU + 12288, F32, [NT, 32])
    M1 = view(Ba, SU + 14336, F32, [NT, 32])
    for tab, shift in ((SIN, 0.0), (COS, math.pi / 2.0)):
        ts(A2, ANG, shift, ALU.add)
        ts(KF, A2, 1.0 / TWO_PI, ALU.mult)
        cp(KI, KF)
        cp(KF, KI)
        stt(YR, KF, -TWO_PI, A2, ALU.mult, ALU.add)
        ts(M1, YR, math.pi, ALU.is_gt)
        stt(YR, M1, -TWO_PI, YR, ALU.mult, ALU.add)
        ts(M1, YR, -math.pi, ALU.is_lt)
        stt(YR, M1, TWO_PI, YR, ALU.mult, ALU.add)
        ts(YR, YR, -math.pi, ALU.max, math.pi, ALU.min)
        act(tab, YR, AF.Sin)

    def rms_rstd(ss, n, rstd_out):
        ts(RTMP[:, 0:n], ss[:, 0:n], 1.0 / D, ALU.mult, EPS, ALU.add)
        act(RTMP2[:, 0:n], RTMP[:, 0:n], AF.Sqrt)
        recip(rstd_out[:, 0:n], RTMP2[:, 0:n])

    def make_hT(t, rstd_col, which, dstT, tloc, act_evac=False):
        pb = bf(bank())
        for hf in range(2):
            xh = XN[:, hf * 512:(hf + 1) * 512]
            act(xh, X[:, t, hf * 512:(hf + 1) * 512], AF.Copy, scale=rstd_col)
            for k in range(4):
                tr(pb[:, (4 * hf + k) * 128:(4 * hf + k + 1) * 128], xh[:, k * 128:(k + 1) * 128])
        if act_evac:
            for k in range(8):
                act(dstT[:, k, tloc * 128:(tloc + 1) * 128], pb[:, k * 128:(k + 1) * 128], AF.Copy,
                    scale=GPRE[:, which, k:k + 1])
        else:
            tt(dstT[:, :, tloc * 128:(tloc + 1) * 128], pb.rearrange("p (k c) -> p k c", c=128),
               GPRE[:, which, :].unsqueeze(2).to_broadcast([128, 8, 128]), ALU.mult)

    def post_norm_sq(t, pair):
        act(JUNK, pair, AF.Square, accum=SSP[:, t:t + 1])

    def post_norm_rstd(t):
        ts(PT1[:, t:t + 1], SSP[:, t:t + 1], 1.0 / D, ALU.mult, EPS, ALU.add)
        act(PT2[:, t:t + 1], PT1[:, t:t + 1], AF.Sqrt)
        recip(RSTDP[:, t:t + 1], PT2[:, t:t + 1])

    def post_norm_update(t, pair, idx):
        post_norm_sq(t, pair)
        post_norm_rstd(t)

    def post_norm_apply(t, pair, tmpbufs, next_ss=None, next_rstd=None, wide=None):
        if wide is not None:
            stt(wide, pair, RSTDP[:, t:t + 1], GPOST, ALU.mult, ALU.mult)
            tt(X[:, t, :], X[:, t, :], wide, ALU.add)
        else:
            for hf in range(2):
                tmp = tmpbufs[hf]
                stt(tmp, pair[:, hf * 512:(hf + 1) * 512], RSTDP[:, t:t + 1], GPOST[:, hf * 512:(hf + 1) * 512],
                    ALU.mult, ALU.mult)
                tt(X[:, t, hf * 512:(hf + 1) * 512], X[:, t, hf * 512:(hf + 1) * 512], tmp, ALU.add)
        if next_ss is not None:
            act(JUNK, X[:, t, :], AF.Square, accum=next_ss[:, t:t + 1])
        if next_rstd is not None:
            ts(RTMP[:, t:t + 1], next_ss[:, t:t + 1], 1.0 / D, ALU.mult, EPS, ALU.add)
            act(RTMP2[:, t:t + 1], RTMP[:, t:t + 1], AF.Sqrt)
            recip(next_rstd[:, t:t + 1], RTMP2[:, t:t + 1])

    def win_block(l, c0):
        s = next_slot()
        g = _Grp()
        sv = SLOT[s].rearrange("p (k c) -> p k c", c=512)
        dma("pool", sv, w_in[l, :, c0:c0 + 512].rearrange("(k p) c -> p k c", p=128), "ring%d" % s, g)
        return sv

    def stop_at(name):
        if stop == name:
            raise _Stop()

    def layer(l):
        gg = _Grp()
        dma("sp", GPRE[:, 0, :], g_mpre[l].rearrange("(k p) -> p k", p=128), "gpre", gg, noncontig=True)
        dma("sp", GPRE[:, 1, :], g_fpre[l].rearrange("(k p) -> p k", p=128), "gpre", gg, noncontig=True)

        if l == 0:
            for t in range(NT):
                act(JUNK, X[:, t, :], AF.Square, accum=SS[:, t:t + 1])
        rms_rstd(SS, NT, RSTD_MIX)
        dump("hT%d" % l, HT, [8, S])

        stop_at('N1')
        memset(VA[:, :, :, 64:72], 0.0, q="pool")
        memset(VA[:, :, :, 64:65], 1.0, q="pool")
        svq = win_block(l, O_Q)
        svk = win_block(l, O_K)
        rope_state = {}

        def a_mm(t):
            pbs = []
            for sv in (svq, svk):
                pb = bank()
                for k in range(8):
                    mm(pb, HT[:, k, t * 128:(t + 1) * 128], sv[:, k, :], start=(k == 0), stop=(k == 7))
                pbs.append(pb)
            cosb = COS[:, t, :].unsqueeze(1).unsqueeze(1).to_broadcast([128, 8, 2, 32])
            sinb = SIN[:, t, :].unsqueeze(1).to_broadcast([128, 8, 32])
            p4 = [pb.rearrange("p (h two d) -> p h two d", two=2, d=32) for pb in pbs]
            t1 = [RT1[i].rearrange("p (h two d) -> p h two d", two=2, d=32) for i in range(2)]
            t2 = [RT2[i].rearrange("p (h two d) -> p h two d", two=2, d=32) for i in range(2)]
            for i in range(2):
                tt(t1[i], p4[i], cosb, ALU.mult)
            for i in range(2):
                stt(t2[i][:, :, 0, :], p4[i][:, :, 1, :], -1.0, sinb, ALU.mult, ALU.mult)
            for i in range(2):
                tt(t2[i][:, :, 1, :], p4[i][:, :, 0, :], sinb, ALU.mult)
            for i in range(2):
                tt(QR[2 * (t % 2) + i], RT1[i], RT2[i], ALU.add)

        def a_tr(t):
            for i in range(2):
                qr = QR[2 * (t % 2) + i]
                pt = bf(bank())
                for j in range(4):
                    tr(pt[:, j * 128:(j + 1) * 128], qr[:, j * 128:(j + 1) * 128])
                dst = (QT if i == 0 else KT)[:, :, t * 128:(t + 1) * 128]
                act(dst, pt[:, 0:512].rearrange("p (j c) -> p j c", c=128), AF.Copy,
                    scale=(0.125 if i == 0 else 1.0))

        for i_ in range(NT + 3):
            if i_ < NT:
                make_hT(i_, RSTD_MIX[:, i_:i_ + 1], 0, HT, i_, act_evac=True)
            if 2 <= i_ < NT + 2:
                a_mm(i_ - 2)
            if i_ >= 3:
                a_tr(i_ - 3)
        for pr in range(4):
            P.add("dve", (lambda pr=pr: nc.vector.tensor_reduce(
                out=KBAR[:, pr, :], in_=KT[:, pr, :].rearrange("p (n s) -> p n s", s=256), axis=AX.X, op=ALU.add)),
                reads=[KT[:, pr, :]], writes=[KBAR[:, pr, :]])
        cp(KBH, KBAR)
        tt(KBAR2, KBAR, KBH, ALU.subtract)
        cp(KBL, KBAR2)
        sv = win_block(l, O_VA)
        for t in range(NT):
            pb = bank()
            for k in range(8):
                mm(pb, HT[:, k, t * 128:(t + 1) * 128], sv[:, k, :], start=(k == 0), stop=(k == 7))
            act(VA[:, t, :, 0:64], pb.rearrange("p (h d) -> p h d", d=64), AF.Copy)
        dump("qT%d" % l, QT, [4, S])
        dump("kT%d" % l, KT, [4, S])
        dump("va%d" % l, VA, [NT, 8, 72])

        stop_at('A')

        stop_at('B0')

        def gates(b):
            for qi in range(2):
                qt = 2 * b + qi
                gbs = [bank(), bank()]
                for par in range(2):
                    lo = 64 * par
                    for pr in range(4):
                        o = gbs[par][:, pr * 8:(pr + 1) * 8]
                        mm(o, QT[lo:lo + 64, pr, qt * 128:(qt + 1) * 128], KBH[lo:lo + 64, pr, :], start=True, stop=False)
                        mm(o, QT[lo:lo + 64, pr, qt * 128:(qt + 1) * 128], KBL[lo:lo + 64, pr, :], start=False, stop=True)
                g4 = GSB.rearrange("p (hp two) n -> p hp two n", two=2)
                for par in range(2):
                    cp(g4[:, :, par, :], gbs[par][:, 0:32].rearrange("p (h n) -> p h n", n=8))
                memset(GSB[:, :, b:8], -1e30)
                for h in range(8):
                    P.add("dve", (lambda h=h: nc.vector.max(out=TOP8[:, h, :], in_=GSB[:, h, :])),
                          reads=[GSB[:, h, :]], writes=[TOP8[:, h, :]])
                tt(CMP, GSB, TOP8[:, :, 2:3].to_broadcast([128, 8, 8]), ALU.is_lt)
                ts(MBTOK.rearrange("p (r c) -> p r c", c=64),
                   CMP.rearrange("p h n -> p (h n)").unsqueeze(1).to_broadcast([128, 2, 64]), NEG, ALU.mult)
                pm = bf(bank())
                tr(pm[:, 0:128], MBTOK)
                cp(MBT[:, (qt - 8) * 128:(qt - 8 + 1) * 128], pm[:, 0:128])

        def score_tiles(b, pr, ub):
            PT = PTB[ub]
            tiles = []

            def past_mm(m, st):
                sbs = [bank(), bank()]
                st["sbs"] = sbs
                for j in range(2):
                    kt = 2 * m + j
                    for par in range(2):
                        lo = 64 * par
                        mm(sbs[par][:, j * 256:(j + 1) * 256], KT[lo:lo + 64, pr, kt * 128:(kt + 1) * 128],
                           QT[lo:lo + 64, pr, b * 256:(b + 1) * 256], start=True, stop=(b < 4))
                    if b >= 4:
                        for par in range(2):
                            lo = 64 * par
                            h = 2 * pr + par
                            sel = IDENT[lo:lo + 64, lo + h * 8 + m:lo + h * 8 + m + 1].to_broadcast([64, 128])
                            mm(sbs[par][:, j * 256:(j + 1) * 256], sel,
                               MBT[lo:lo + 64, (b - 4) * 256:(b - 4 + 1) * 256], start=False, stop=True)

            def past_exp(m, st):
                sbs = st["sbs"]
                for par in range(2):
                    act(PT[par][:, 2 * m:2 * m + 2, :], sbs[par].rearrange("p (j c) -> p j c", c=256), AF.Exp)

            def own_mm(st):
                sbs = [bank(), bank()]
                st["sbs"] = sbs
                kt = 2 * b
                for par in range(2):
                    lo = 64 * par
                    mm(sbs[par][:, 0:128], KT[lo:lo + 64, pr, kt * 128:(kt + 1) * 128],
                       QT[lo:lo + 64, pr, b * 256:b * 256 + 128], start=True, stop=False)
                for par in range(2):
                    mm(sbs[par][:, 0:128], IDENT, TRI, start=False, stop=True)
                for par in range(2):
                    lo = 64 * par
                    mm(sbs[par][:, 128:256], KT[lo:lo + 64, pr, kt * 128:(kt + 1) * 128],
                       QT[lo:lo + 64, pr, b * 256 + 128:b * 256 + 256], start=True, stop=True)
                kt = 2 * b + 1
                for par in range(2):
                    lo = 64 * par
                    mm(sbs[par][:, 256:384], KT[lo:lo + 64, pr, kt * 128:(kt + 1) * 128],
                       QT[lo:lo + 64, pr, b * 256 + 128:b * 256 + 256], start=True, stop=False)
                for par in range(2):
                    mm(sbs[par][:, 256:384], IDENT, TRI, start=False, stop=True)

            def own_exp(st):
                sbs = st["sbs"]
                for par in range(2):
                    act(PT[par][:, 2 * b, :], sbs[par][:, 0:256], AF.Exp)
                    act(PT[par][:, 2 * b + 1, 128:256], sbs[par][:, 256:384], AF.Exp)

            for m in range(b):
                st = {}
                tiles.append(((lambda m=m, st=st: past_mm(m, st)), (lambda m=m, st=st: past_exp(m, st))))
            st = {}
            tiles.append(((lambda st=st: own_mm(st)), (lambda st=st: own_exp(st))))
            return tiles

        def pv(b, h, PTv, ybuf):
            ab = bank()
            for qi in range(2):
                nk = 2 * b + qi + 1
                for kt in range(nk):
                    mm(ab[:, qi * 128:qi * 128 + 66], PTv[:, kt, qi * 128:(qi + 1) * 128], VA[:, kt, h, 0:66],
                       start=(kt == 0), stop=(kt == nk - 1))
            a3 = ab[:, 0:256].rearrange("p (q c) -> p q c", c=128)
            rc = REC[h % 2]
            recip(rc, a3[:, :, 64])
            tt(YBTOK[ybuf][:, :, h * 64:(h + 1) * 64], a3[:, :, 0:64], rc.unsqueeze(2).to_broadcast([128, 2, 64]),
               ALU.mult)

        def yb_transpose(b, ybuf):
            pt = bf(bank())
            p3 = pt.rearrange("p (c q) -> p c q", q=256)
            for qi in range(2):
                for c in range(4):
                    tr(p3[:, c, qi * 128:(qi + 1) * 128], YBTOK[ybuf][:, qi, c * 128:(c + 1) * 128])
            cp(YBT[:, :, b * 256:(b + 1) * 256], p3)

        units = [(b, pr) for b in range(8) for pr in range(4)]

        def pv_unit(i):
            pb_, ppr = units[i]
            for par in range(2):
                pv(pb_, 2 * ppr + par, PTB[i % 2][par], pb_ % 2)
            if ppr == 3:
                yb_transpose(pb_, pb_ % 2)

        for i, (b, pr) in enumerate(units):
            if pr == 0 and b >= 4:
                gates(b)
            for fm, fe in score_tiles(b, pr, i % 2):
                fm()
                fe()
            if i >= 1:
                pv_unit(i - 1)
        pv_unit(len(units) - 1)
        dump("ybT%d" % l, YBT, [4, S])
        dump("mbt%d" % l, MBT, [1024])

        stop_at('B')
        for t in range(NT):
            make_hT(t, RSTD_MIX[:, t:t + 1], 0, HT, t)

        stop_at('N2')
        gc = _Grp()
        dma("sp", LNGc, ln_g[l].rearrange("(gp p) -> p gp", p=128), "lng", gc, noncontig=True)
        gc2 = _Grp()
        dma("sp", LNBc, ln_b[l].rearrange("(gp p) -> p gp", p=128), "lnb", gc2, noncontig=True)
        gc3 = _Grp()
        for g in range(8):
            dma("sp", BSB[64 * (g % 2):64 * (g % 2) + 64, g // 2, :], b_s[l, g].partition_broadcast(64), "bsb", gc3)
        gc4 = _Grp()
        dma("sp", WSS, w_s[l].rearrange("g i j -> i g j"), "wss", gc4)
        cp(WS16, WSS)
        pw = bf(bank())
        for g in range(8):
            tr(pw[:, g * 128:(g + 1) * 128], WS16[:, g, :])
        tt(WST, pw.rearrange("p (g c) -> p g c", c=128), MASK01.unsqueeze(1).to_broadcast([128, 8, 128]), ALU.mult)
        prs = bank()
        for gp in range(4):
            for e in range(2):
                mm(prs[64 * e:64 * e + 64, gp * 128:(gp + 1) * 128], ONES16[:, 0:64], WST[:, 2 * gp + e, :])
        for gp in range(4):
            stt(BIAS2[:, gp, :], prs[:, gp * 128:(gp + 1) * 128], LNBc[:, gp:gp + 1], BSB[:, gp, :], ALU.mult, ALU.add)

        sv = win_block(l, O_U)
        for m in range(4):
            for tg in range(4):
                pb = bank()
                for k in range(8):
                    mm(pb, sv[:, k, m * 128:(m + 1) * 128], HT[:, k, tg * 512:(tg + 1) * 512], start=(k == 0), stop=(k == 7))
                act(UT[:, m, tg * 512:(tg + 1) * 512], pb, AF.Gelu)
        sv = win_block(l, O_VG)

        def vg_stage1(g):
            for t in range(4 * g, 4 * g + 4):
                i = t % 8
                pb = bank()
                for k in range(8):
                    mm(pb, HT[:, k, t * 128:(t + 1) * 128], sv[:, k, :], start=(k == 0), stop=(k == 7))
                act(G32[i], pb, AF.Gelu)
            for t in range(4 * g, 4 * g + 4):
                i = t % 8
                P.add("dve", (lambda i=i: nc.vector.bn_stats(out=BNST[i], in_=G32[i])), reads=[G32[i]], writes=[BNST[i]])
            for t in range(4 * g, 4 * g + 4):
                i = t % 8
                P.add("dve", (lambda i=i: nc.vector.bn_aggr(out=MV8[:, i, :], in_=BNST[i])), reads=[BNST[i]],
                      writes=[MV8[:, i, :]])

        def vg_stage23(g):
            i0 = (4 * g) % 8
            ts(LNT4[:, i0:i0 + 4], MV8[:, i0:i0 + 4, 1], EPS, ALU.add)
            act(LNT24[:, i0:i0 + 4], LNT4[:, i0:i0 + 4], AF.Sqrt)
            recip(LNR4[:, i0:i0 + 4], LNT24[:, i0:i0 + 4])
            tl = list(range(4 * g, 4 * g + 4))
            for t in tl:
                i = t % 8
                ts(V16[t % 4], G32[i], MV8[:, i, 0:1], ALU.subtract, LNR4[:, i:i + 1], ALU.mult)
            pbs = {}
            for t in tl:
                pb = bank()
                pbs[t] = pb
                for gp in range(4):
                    for e in range(2):
                        gg_ = 2 * gp + e
                        mm(pb[64 * e:64 * e + 64, gp * 128:(gp + 1) * 128], V16[t % 4][:, gg_ * 64:(gg_ + 1) * 64],
                           WST[:, gg_, :])
            for t in tl:
                g3 = G32[t % 8].rearrange("p (a b) -> p a b", b=128)
                for gp in range(4):
                    stt(g3[:, gp, :], pbs[t][:, gp * 128:(gp + 1) * 128], LNGc[:, gp:gp + 1], BIAS2[:, gp, :],
                        ALU.mult, ALU.add)
            for t in tl:
                ya = UT[:, :, t * 128:(t + 1) * 128]
                tt(ya, G32[t % 8].rearrange("p (a b) -> p a b", b=128), ya, ALU.mult)

        for g in range(5):
            if g < 4:
                vg_stage1(g)
            if g >= 1:
                vg_stage23(g - 1)
        dump("yaT%d" % l, UT, [4, S])

        stop_at('C1')
        for m in range(8):
            s = next_slot()
            g = _Grp()
            sl = SLOT[s]
            GAv = sl[:, 0:1024].rearrange("p (k c) -> p k c", c=128)
            GBv = sl[:, 1024:2048].rearrange("p (k c) -> p k c", c=128)
            WAv = sl[:, 2048:2560].rearrange("p (k c) -> p k c", c=128)
            WBv = sl[:, 2560:3072].rearrange("p (k c) -> p k c", c=128)
            sem = "ring%d" % s
            dma("pool", GAv, w_in[l, :, O_GA + m * 128:O_GA + (m + 1) * 128].rearrange("(k p) c -> p k c", p=128), sem, g)
            dma("pool", GBv, w_in[l, :, O_GB + m * 128:O_GB + (m + 1) * 128].rearrange("(k p) c -> p k c", p=128), sem, g)
            dma("pool", WAv, w_pa[l, :, m * 128:(m + 1) * 128].rearrange("(k p) c -> p k c", p=128), sem, g)
            dma("pool", WBv, w_pb[l, :, m * 128:(m + 1) * 128].rearrange("(k p) c -> p k c", p=128), sem, g)
            for tg in range(4):
                tok = slice(tg * 512, (tg + 1) * 512)
                bA, bB, bC, bD = bank(), bank(), bank(), bank()
                for k in range(8):
                    mm(bB, GAv[:, k, :], HT[:, k, tok], start=(k == 0), stop=(k == 7))
                for k in range(4):
                    mm(bA, WAv[:, k, :], UT[:, k, tok], start=(k == 0), stop=(k == 3))
                for k in range(8):
                    mm(bD, GBv[:, k, :], HT[:, k, tok], start=(k == 0), stop=(k == 7))
                for k in range(4):
                    mm(bC, WBv[:, k, :], YBT[:, k, tok], start=(k == 0), stop=(k == 3))
                act(SGA, bB, AF.Sigmoid)
                act(SGB, bD, AF.Sigmoid)
                tt(T1, bA, SGA, ALU.mult)
                tt(T2, bC, SGB, ALU.mult)
                tt(MT[:, m, tok], T1, T2, ALU.add)
        dump("mT%d" % l, MT, [8, S])

        stop_at('C2')
        gp_ = _Grp()
        dma("sp", GPOST, g_mpost[l].partition_broadcast(128), "gpost", gp_)
        wo = []
        for nb in range(2):
            s = next_slot()
            g = _Grp()
            svo = SLOT[s].rearrange("p (k c) -> p k c", c=512)
            dma("pool", svo, w_out[l, :, nb * 512:(nb + 1) * 512].rearrange("(k p) c -> p k c", p=128), "ring%d" % s, g)
            wo.append(svo)
        pairs = {}
        for t in range(NT + 2):
            if t < NT:
                pr_ = bank_pair(keep=3)
                pairs[t] = pr_
                for nb in range(2):
                    for k in range(8):
                        mm(pr_[:, nb * 512:(nb + 1) * 512], MT[:, k, t * 128:(t + 1) * 128], wo[nb][:, k, :],
                           start=(k == 0), stop=(k == 7))
            if t >= 2:
                post_norm_apply(t - 2, pairs[t - 2], TMPC, next_ss=SSF, wide=TMPCW[t % 2])
            if 1 <= t <= NT:
                post_norm_rstd(t - 1)
            if t < NT:
                post_norm_sq(t, pairs[t])
        live_pair[0] = None
        rms_rstd(SSF, NT, RSTD_F)
        for tl in range(8):
            make_hT(tl, RSTD_F[:, tl:tl + 1], 1, HTH, tl)
        live_pair[0] = None
        dump("xmid%d" % l, X, [NT, D])

        stop_at('C3')
        for hf in range(2):
            for ffp in range(NFF // 2):
                s = next_slot()
                g = _Grp()
                sv = SLOT[s].rearrange("p (k c) -> p k c", c=512)
                sem = "ring%d" % s
                dma("pool", sv[:, :, 0:256], w_gu[l, :, ffp * 256:(ffp + 1) * 256].rearrange("(k p) c -> p k c", p=128), sem, g)
                dma("pool", sv[:, :, 256:512],
                    w_gu[l, :, DFF + ffp * 256:DFF + (ffp + 1) * 256].rearrange("(k p) c -> p k c", p=128), sem, g)
                if hf == 0 and ffp == 2:
                    gw = _Grp()
                    for (k0, k1) in ((0, 6), (6, 12), (12, 18), (18, 22)):
                        dma("pool", WD[:, k0:k1, :], w_dn[l, k0 * 128:k1 * 128, :].rearrange("(k p) c -> p k c", p=128), "wd", gw)
                    gq = _Grp()
                    dma("sp", GPOST, g_fpost[l].partition_broadcast(128), "gpost", gq)
                for c in range(2):
                    ffc = 2 * ffp + c
                    for tg in range(2):
                        tok = slice(tg * 512, (tg + 1) * 512)
                        bG, bU = bank(), bank()
                        for k in range(8):
                            mm(bG, sv[:, k, c * 128:(c + 1) * 128], HTH[:, k, tok], start=(k == 0), stop=(k == 7))
                        for k in range(8):
                            mm(bU, sv[:, k, 256 + c * 128:256 + (c + 1) * 128], HTH[:, k, tok], start=(k == 0), stop=(k == 7))
                        si = (ffc * 2 + tg) % 2
                        act(SL[si], bG, AF.Silu)
                        tt(ACTT[:, ffc, tok], bU, SL[si], ALU.mult)
            if hf == 0:
                for tl in range(8):
                    make_hT(8 + tl, RSTD_F[:, 8 + tl:8 + tl + 1], 1, HTH, tl)
            pairs = {}
            for tl in range(9):
                if tl < 8:
                    t = 8 * hf + tl
                    pr_ = bank_pair()
                    pairs[tl] = pr_
                    for nb in range(2):
                        for k in range(NFF):
                            mm(pr_[:, nb * 512:(nb + 1) * 512], ACTT[:, k, tl * 128:(tl + 1) * 128],
                               WD[:, k, nb * 512:(nb + 1) * 512], start=(k == 0), stop=(k == NFF - 1))
                    post_norm_update(t, pr_, t)
                if tl >= 1:
                    post_norm_apply(8 * hf + tl - 1, pairs[tl - 1], TMPF, next_ss=(SS if l + 1 < L else None))
            live_pair[0] = None
        dump("xout%d" % l, X, [NT, D])


    try:
        stop_at('setup')
        for l in range(L):
            layer(l)
    except _Stop:
        pass

    go = _Grp()
    for t in range(NT):
        dma("sp", out[t * 128:(t + 1) * 128, :], X[:, t, :], "ost", go)
    P.add("sp", lambda: nc.sync.nop() if hasattr(nc.sync, "nop") else nc.sync.engine_nop(), reads=[], writes=[])
    fin = P.ops[-1]
    fin.waits = [go.ops[-1]]
    nsem = P.emit()
    return nc, dbg_outs, len(P.ops), nsem


_CACHE = {}


def _get_prog(nlayers):
    if nlayers not in _CACHE:
        _CACHE[nlayers] = build_program(nlayers)
    return _CACHE[nlayers]


W_NAMES = ["w_in", "w_s", "b_s", "ln_v_g", "ln_v_b", "w_proj_a", "w_proj_b", "w_out", "g_mix_pre", "g_mix_post",
           "g_ffn_pre", "g_ffn_post", "w_gate_up", "w_down"]


def kernel(x, positions, **w):
    x = np.asarray(x, dtype=np.float32)
    positions = np.asarray(positions, dtype=np.int32)
    ws = {k: np.ascontiguousarray(np.asarray(w[k], dtype=np.float32)) for k in W_NAMES}
    nc, _, _, _ = _get_prog(DEPTH)
    in_maps = []
    for c in range(NCORES):
        m = {"x": np.ascontiguousarray(x[c]), "positions": np.ascontiguousarray(positions[c])}
        m.update(ws)
        in_maps.append(m)
    res = run_bass_kernel_spmd(nc, in_maps, core_ids=list(range(NCORES)))
    return np.stack([np.asarray(res.results[c]["out"], dtype=np.float32) for c in range(NCORES)], axis=0)
```
